# Optimizing a Trainium2 kernel written in Bass

```python
import jax, jax.numpy as jnp
from jax import lax
import numpy as np

D_MODEL = 4096
BATCH = 4
SEQ = 2048
DEPTH = 2

D_MIX = D_MODEL
RET_HEADS = 16
D_RET = D_MIX // 2
RET_HEAD_DIM = D_RET // RET_HEADS
D_CONV = D_MIX - D_RET
CONV_GROUPS = 16
CONV_WIDTH = 3
D_FF = 11008
CHUNK = 128
ROPE_THETA = 10000.0
EPS = 1e-6
D_IN_PROJ = 4 * D_RET + 3 * D_CONV

kernel_name = "hybrid_retention_shortconv_macaron"


def rmsnorm(x, g):
    xf = x.astype(jnp.float32)
    y = xf * lax.rsqrt(jnp.mean(xf * xf, axis=-1, keepdims=True) + EPS)
    return (y * g.astype(jnp.float32)).astype(x.dtype)


def swiglu(x, w_gate, w_up, w_down):
    return (jax.nn.silu(x @ w_gate) * (x @ w_up)) @ w_down


def rope(x):
    s, dh = x.shape[1], x.shape[-1]
    half = dh // 2
    inv_freq = ROPE_THETA ** (-jnp.arange(half, dtype=jnp.float32) / half)
    ang = jnp.arange(s, dtype=jnp.float32)[:, None] * inv_freq[None, :]
    cos = jnp.cos(ang)[None, :, None, :]
    sin = jnp.sin(ang)[None, :, None, :]
    x1, x2 = x[..., :half], x[..., half:]
    return jnp.concatenate([x1 * cos - x2 * sin, x1 * sin + x2 * cos], axis=-1)


def retention_chunkwise(q, k, v):
    b, s, h, dh = q.shape
    n = s // CHUNK
    log_gamma = jnp.log1p(-jnp.power(2.0, -5.0 - jnp.arange(h, dtype=jnp.float32)))

    def to_chunks(t):
        return t.reshape(b, n, CHUNK, h, dh).transpose(0, 3, 1, 2, 4)

    qc, kc, vc = to_chunks(q), to_chunks(k), to_chunks(v)
    idx = jnp.arange(CHUNK, dtype=jnp.float32)
    diff = idx[:, None] - idx[None, :]
    intra_decay = jnp.where(diff[None] >= 0,
                            jnp.exp(log_gamma[:, None, None] * jnp.maximum(diff, 0.0)[None]),
                            0.0)
    zeta = jnp.exp(log_gamma[:, None] * (CHUNK - 1.0 - idx)[None, :])
    xi = jnp.exp(log_gamma[:, None] * (idx + 1.0)[None, :])
    chunk_decay = jnp.exp(log_gamma * CHUNK)

    scores = jnp.einsum('bhncd,bhnmd->bhncm', qc, kc) * intra_decay[None, :, None]
    out_intra = jnp.einsum('bhncm,bhnme->bhnce', scores, vc)

    kv_chunk = jnp.einsum('bhnmd,bhnme->bhnde', kc * zeta[None, :, None, :, None], vc)

    def step(state, kv_n):
        return state * chunk_decay[None, :, None, None] + kv_n, state

    _, prev_states = lax.scan(step, jnp.zeros((b, h, dh, dh), jnp.float32),
                              jnp.moveaxis(kv_chunk, 2, 0))
    prev_states = jnp.moveaxis(prev_states, 0, 2)
    out_inter = jnp.einsum('bhncd,bhnde->bhnce', qc, prev_states) * xi[None, :, None, :, None]

    out = out_intra + out_inter
    return out.transpose(0, 2, 3, 1, 4).reshape(b, s, h, dh)


def causal_depthwise_conv3(u, w):
    s = u.shape[1]
    up = jnp.pad(u, ((0, 0), (CONV_WIDTH - 1, 0), (0, 0)))
    return sum(w[j][None, None, :] * up[:, j:j + s, :] for j in range(CONV_WIDTH))


def hybrid_mixer(h, w_in, conv_w, ret_norm, w_out):
    b, s, _ = h.shape
    proj = h @ w_in
    splits = [D_RET, 2 * D_RET, 3 * D_RET, 4 * D_RET, 4 * D_RET + D_CONV, 4 * D_RET + 2 * D_CONV]
    q, k, v, g, gate_b, gate_c, u = jnp.split(proj, splits, axis=-1)

    shp = (b, s, RET_HEADS, RET_HEAD_DIM)
    qf = rope(q.reshape(shp).astype(jnp.float32))
    kf = rope(k.reshape(shp).astype(jnp.float32)) * (RET_HEAD_DIM ** -0.5)
    vf = v.reshape(shp).astype(jnp.float32)
    ret = retention_chunkwise(qf, kf, vf)
    ret = ret * lax.rsqrt(jnp.mean(ret * ret, axis=-1, keepdims=True) + EPS)
    ret = ret.reshape(b, s, D_RET) * ret_norm.astype(jnp.float32)
    ret_out = (jax.nn.silu(g.astype(jnp.float32)) * ret).astype(h.dtype)

    conv_out = gate_b * causal_depthwise_conv3(gate_c * u, conv_w)

    return jnp.concatenate([ret_out, conv_out], axis=-1) @ w_out


def setup_inputs(seed: int = 0) -> dict:
    key = jax.random.key(seed)
    ks = jax.random.split(key, 16)
    f32 = jnp.float32

    def nrm(k, shape, scale):
        return jax.random.normal(k, shape, f32) * scale

    def gain(k, shape):
        return 1.0 + 0.02 * jax.random.normal(k, shape, f32)

    return {
        "x": nrm(ks[0], (BATCH, SEQ, D_MODEL), 1.0),
        "norm_ffa": gain(ks[1], (DEPTH, D_MODEL)),
        "w_ffa_gate": nrm(ks[2], (DEPTH, D_MODEL, D_FF), D_MODEL ** -0.5),
        "w_ffa_up": nrm(ks[3], (DEPTH, D_MODEL, D_FF), D_MODEL ** -0.5),
        "w_ffa_down": nrm(ks[4], (DEPTH, D_FF, D_MODEL), D_FF ** -0.5),
        "norm_mix": gain(ks[5], (DEPTH, D_MODEL)),
        "w_in": nrm(ks[6], (DEPTH, D_MODEL, D_IN_PROJ), D_MODEL ** -0.5),
        "conv_w": nrm(ks[7], (DEPTH, CONV_WIDTH, D_CONV), CONV_WIDTH ** -0.5),
        "ret_norm": gain(ks[8], (DEPTH, D_RET)),
        "w_out": nrm(ks[9], (DEPTH, D_MIX, D_MODEL), D_MIX ** -0.5),
        "norm_ffb": gain(ks[10], (DEPTH, D_MODEL)),
        "w_ffb_gate": nrm(ks[11], (DEPTH, D_MODEL, D_FF), D_MODEL ** -0.5),
        "w_ffb_up": nrm(ks[12], (DEPTH, D_MODEL, D_FF), D_MODEL ** -0.5),
        "w_ffb_down": nrm(ks[13], (DEPTH, D_FF, D_MODEL), D_FF ** -0.5),
        "norm_final": gain(ks[14], (D_MODEL,)),
    }


def reference(x, norm_ffa, w_ffa_gate, w_ffa_up, w_ffa_down, norm_mix, w_in, conv_w,
              ret_norm, w_out, norm_ffb, w_ffb_gate, w_ffb_up, w_ffb_down, norm_final):
    for l in range(DEPTH):
        x = x + 0.5 * swiglu(rmsnorm(x, norm_ffa[l]), w_ffa_gate[l], w_ffa_up[l], w_ffa_down[l])
        x = x + hybrid_mixer(rmsnorm(x, norm_mix[l]), w_in[l], conv_w[l], ret_norm[l], w_out[l])
        x = x + 0.5 * swiglu(rmsnorm(x, norm_ffb[l]), w_ffb_gate[l], w_ffb_up[l], w_ffb_down[l])
    return rmsnorm(x, norm_final)
```

```python
import numpy as np
from contextlib import ExitStack
import concourse.bass as bass
import concourse.mybir as mybir
from concourse.bass_utils import run_bass_kernel_spmd

F32 = mybir.dt.float32
BF16 = mybir.dt.bfloat16
ALU = mybir.AluOpType
AF = mybir.ActivationFunctionType
EPS = 1e-6
NCORES = 8


class StopBuild(Exception):
    pass


class Cfg:
    def __init__(s, D=4096, FF=11008, DEPTH=2, TT=1024, mixer=True, debug=False):
        s.debug = debug
        import os
        s.bg2 = bool(int(os.environ.get('BG2', '1')))
        s.bgn = int(os.environ.get('BGN', '1'))
        s.gate = int(os.environ.get('BGGATE', '0'))
        s.pbser = int(os.environ.get('PBSER', '0'))
        s.stopat = int(os.environ.get('STOPAT', '0'))
        s.nobg = int(os.environ.get('NOBG', '0'))
        s.pedrain = int(os.environ.get('PEDRAIN', '0'))
        s.bgsteps = int(os.environ.get('BGSTEPS', '100000'))
        s.bgskip = int(os.environ.get('BGSKIP', '0'))
        s.kvbar = int(os.environ.get('KVBAR', '0'))
        s.kvps = int(os.environ.get('KVPS', '0'))
        s.convlate = bool(int(os.environ.get('CONVLATE', '0')))
        s.D, s.FF, s.DEPTH, s.TT = D, FF, DEPTH, TT
        s.KC = D // 128
        s.FC = FF // 128
        s.H = D // 256
        s.NP = 7 * s.H
        s.NCH = TT // 128
        s.NH = TT // 512
        npart = 3
        base, rem = divmod(s.FC, npart)
        s.parts = [base + (1 if i < rem else 0) for i in range(npart)]
        s.PSMAX = max(s.parts)
        s.mixer = mixer
        s.EW = s.H * 128 + 2 * s.H
        o = 0
        def take(n):
            nonlocal o
            r = o
            o += n
            return r
        s.c_ident = take(128)
        s.c_perm = take(128)
        s.c_onesd = take(128)
        s.c_onesh = take(128)
        s.c_cos = take(TT)
        s.c_sin = take(TT)
        s.c_dm = take(s.H * 128)
        s.c_xi = take(s.H * 128)
        s.c_zeta = take(s.H)
        s.c_sel = take(1)
        s.c_eps = take(1)
        s.c_norm = take((3 * DEPTH + 1) * s.KC)
        s.c_rn = take(DEPTH * s.H)
        s.c_cw = take(DEPTH * 3 * s.H)
        s.CW = o
        s.WSLOT = max(2 * s.KC * 128, s.PSMAX * 128)


class Prog:
    ENG = ("pe", "act", "dve", "pool", "sp")

    def __init__(s, nc, es):
        s.nc, s.es = nc, es
        s.ops = {e: [] for e in s.ENG}
        s.sem = {e: es.enter_context(nc.semaphore("ms_" + e)) for e in ("pe", "act", "dve")}
        s.cnt = {e: 0 for e in s.ENG}
        s.waited = {e: {} for e in s.ENG}
        s.dsem, s.dcnt = {}, {}
        s.lastop = {e: None for e in s.ENG}
        s.pending_dma = []

    def dma_sem(s, name):
        s.dsem[name] = s.es.enter_context(s.nc.semaphore(name))
        s.dcnt[name] = 0
        return name

    def _wait(s, eng, tok):
        if tok is None:
            return
        kind, key, val = tok
        if eng == "pe" and kind == "ms" and key == "pe":
            return
        w = s.waited[eng]
        if w.get((kind, key), 0) >= val:
            return
        w[(kind, key)] = val
        s.ops[eng].append(["wait", kind, key, val])

    def _wait_all(s, eng, deps):
        best = {}
        for d in deps:
            for t in (d if isinstance(d, list) else [d]):
                if t is None:
                    continue
                k = (t[0], t[1])
                if k not in best or best[k][2] < t[2]:
                    best[k] = t
        for t in best.values():
            s._wait(eng, t)

    def op(s, eng, fn, deps=(), ms=None, dsem=None, track=True):
        if ms is None:
            ms = eng in ("act", "dve")
        s._wait_all(eng, deps)
        rec = ["op", fn, None]
        tok = None
        if dsem is not None:
            s.dcnt[dsem] += 16
            tok = ("dma", dsem, s.dcnt[dsem])
            rec[2] = ("dma", dsem)
            if track:
                s.pending_dma.append(tok)
        elif ms:
            s.cnt[eng] += 1
            tok = ("ms", eng, s.cnt[eng])
            rec[2] = ("ms", eng)
        s.ops[eng].append(rec)
        s.lastop[eng] = (rec, tok)
        return tok

    def last_tok(s, eng):
        lo = s.lastop[eng]
        if lo is None:
            return None
        rec, tok = lo
        if tok is None and rec[2] is None:
            s.cnt[eng] += 1
            tok = ("ms", eng, s.cnt[eng])
            rec[2] = ("ms", eng)
            s.lastop[eng] = (rec, tok)
        return tok

    def barrier(s):
        toks = [s.last_tok(e) for e in ("pe", "act", "dve")] + s.pending_dma
        s.pending_dma = []
        for e in ("pe", "act", "dve", "sp"):
            s._wait_all(e, [t for t in toks if t is not None and not (t[0] == "ms" and t[1] == e and e == "pe")])

    def simulate(s):
        val = {}
        pos = {e: 0 for e in s.ENG}
        while True:
            prog = False
            for e in s.ENG:
                lst = s.ops[e]
                while pos[e] < len(lst):
                    rec = lst[pos[e]]
                    if rec[0] == "wait":
                        if val.get((rec[1], rec[2]), 0) >= rec[3]:
                            pos[e] += 1
                            prog = True
                        else:
                            break
                    else:
                        if rec[2] is not None:
                            val[rec[2]] = val.get(rec[2], 0) + (1 if rec[2][0] == "ms" else 16)
                        pos[e] += 1
                        prog = True
            if all(pos[e] == len(s.ops[e]) for e in s.ENG):
                return True
            if not prog:
                for e in s.ENG:
                    if pos[e] < len(s.ops[e]):
                        print("DEADLOCK", e, "at", pos[e], "/", len(s.ops[e]), s.ops[e][pos[e]][:4],
                              "cur", val.get((s.ops[e][pos[e]][1], s.ops[e][pos[e]][2])) if s.ops[e][pos[e]][0] == "wait" else "")
                return False

    def emit(s, block):
        def replay(eng, lst):
            def body(e):
                for rec in lst:
                    if rec[0] == "wait":
                        _, kind, key, val = rec
                        sem = s.sem[key] if kind == "ms" else s.dsem[key]
                        e.wait_ge(sem, val)
                    else:
                        ins = rec[1](e)
                        if rec[2] is not None:
                            kind, key = rec[2]
                            if kind == "ms":
                                ins.then_inc(s.sem[key], 1)
                            else:
                                ins.then_inc(s.dsem[key], 16)
            return body
        block.tensor(replay("pe", s.ops["pe"]))
        block.scalar(replay("act", s.ops["act"]))
        block.vector(replay("dve", s.ops["dve"]))
        block.gpsimd(replay("pool", s.ops["pool"]))
        block.sync(replay("sp", s.ops["sp"]))


def build_nc(cfg):
    c = cfg
    D, KC, FC, H, NP, TT, NCH, NH, DEPTH = c.D, c.KC, c.FC, c.H, c.NP, c.TT, c.NCH, c.NH, c.DEPTH
    nc = bass.Bass("TRN2", target_bir_lowering=False)
    es = ExitStack()
    xin = nc.dram_tensor("xin", [KC, 128, TT], F32, kind="ExternalInput")
    consts_d = nc.dram_tensor("consts", [128, c.CW], F32, kind="ExternalInput")
    wgu = {}
    wd = {}
    for l in range(DEPTH):
        for f in "ab":
            wgu[l, f] = nc.dram_tensor(f"wgu_{l}{f}", [FC, 128, 2 * KC * 128], F32, kind="ExternalInput")
            wd[l, f] = nc.dram_tensor(f"wd_{l}{f}", [3 * KC, 128, c.PSMAX * 128], F32, kind="ExternalInput")
    win = [nc.dram_tensor(f"win_{l}", [NP, 128, KC * 128], F32, kind="ExternalInput") for l in range(DEPTH)]
    wout = [nc.dram_tensor(f"wout_{l}", [KC, 128, KC * 128], F32, kind="ExternalInput") for l in range(DEPTH)]
    y = nc.dram_tensor("y", [KC, 128, TT], F32, kind="ExternalOutput")
    XS = nc.dram_tensor("xs", [KC, 128, TT], F32)
    AT = nc.dram_tensor("at", [FC, 128, TT], BF16)
    PT = nc.dram_tensor("pt", [NP, 128, TT], F32, **({"kind": "ExternalOutput"} if c.debug else {}))
    DBR = nc.dram_tensor("dbr", [KC, 128, TT], BF16, kind="ExternalOutput") if c.debug else None
    KVS = nc.dram_tensor("kvs", [H, 128, TT], F32)
    INTRA = nc.dram_tensor("intra", [H, 128, TT], F32)
    QXS = nc.dram_tensor("qxs", [H, 128, TT], BF16)
    CVO = nc.dram_tensor("cvo", [H, 128, TT], BF16)
    EXI = [nc.dram_tensor(f"exi{l}", [128, c.EW], F32) for l in range(DEPTH)]
    EXO = [nc.dram_tensor(f"exo{l}", [256, c.EW], F32) for l in range(DEPTH)]

    P = Prog(nc, es)
    sb = lambda name, shape, dt: es.enter_context(nc.sbuf_tensor(name, shape, dt))
    ACTB = sb("actb", [128, KC, TT], BF16)
    NSLOT = 3
    WB = [sb(f"wb{i}", [128, c.WSLOT], BF16) for i in range(NSLOT)]
    CON = sb("con", [128, c.CW], F32)
    FT = [sb(f"ft{i}", [128, TT], F32) for i in range(7)]
    BT = [sb(f"bt{i}", [128, TT], BF16) for i in range(7)]
    SBALL = sb("sball", [128, TT], BF16)
    SIN = sb("sin", [128, c.EW], F32)
    SST = sb("sst", [128, (NCH + 1) * 128], F32)
    CU = sb("cu", [128, TT + 2], F32)
    HALO = sb("halo", [128, H, 2], F32)
    Y2 = sb("y2", [128, H, 2], F32)
    B2 = sb("b2", [128, H, 2], F32)
    FX = sb("fx", [128, H, 2], F32)
    IDB = sb("idb", [128, 128], BF16)
    PS = [es.enter_context(nc.psum_tensor(f"ps{i}", [128, TT], F32)) for i in range(4)]

    wchunks = []
    for l in range(DEPTH):
        def ffn_chunks(f):
            for fc in range(FC):
                wchunks.append((wgu[l, f][fc, :, :], 2 * KC * 128))
            for pi, ps_ in enumerate(c.parts):
                for dc in range(KC):
                    wchunks.append((wd[l, f][pi * KC + dc, :, 0:ps_ * 128], ps_ * 128))
        ffn_chunks("a")
        if c.mixer:
            for cc in ([H + h for h in range(H)] + [2 * H + h for h in range(H)] + [h for h in range(H)] +
                       [5 * H + j for j in range(H)] + [6 * H + j for j in range(H)] + [4 * H + j for j in range(H)] +
                       [3 * H + h for h in range(H)]):
                wchunks.append((win[l][cc, :, :], KC * 128))
            for dc in range(KC):
                wchunks.append((wout[l][dc, :, :], KC * 128))
        ffn_chunks("b")
    wsem = [P.dma_sem(f"w{i}") for i in range(NSLOT)]
    wstate = {"issued": 0, "consumed": 0, "ltok": {}}

    def w_issue(free_tok):
        i = wstate["issued"]
        if i >= len(wchunks):
            return
        ap, n = wchunks[i]
        slot = i % NSLOT
        tok = P.op("pool", lambda e, ap=ap, n=n, slot=slot: e.dma_start(out=WB[slot][:, 0:n], in_=ap),
                   deps=[free_tok], dsem=wsem[slot], track=False)
        wstate["ltok"][i] = tok
        wstate["issued"] = i + 1

    def w_next():
        i = wstate["consumed"]
        wstate["consumed"] = i + 1
        return i % NSLOT, wstate["ltok"].pop(i)

    for _ in range(NSLOT):
        w_issue(None)

    psfree = [None] * 4

    def mm_group(ps, slot, woff, kcn, rhs_of, deps, mid=None, kdeps=None):
        tok = None
        if mid is not None:
            for hf in range(NH):
                for kc in range(kcn):
                    last = (kc == kcn - 1) and (hf == NH - 1)
                    tok = P.op(
                        "pe",
                        lambda e, kc=kc, hf=hf: e.matmul(
                            out=ps[:, hf * 512:(hf + 1) * 512],
                            lhsT=WB[slot][:, woff + kc * 128: woff + (kc + 1) * 128],
                            rhs=rhs_of(kc)[:, hf * 512:(hf + 1) * 512],
                            start=(kc == 0), stop=(kc == kcn - 1)),
                        deps=deps if (kc == 0 and hf == 0) else (), ms=last)
                if hf < NH - 1:
                    mid()
            return tok
        for kc in range(kcn):
            for hf in range(NH):
                last = (kc == kcn - 1) and (hf == NH - 1)
                d_ = list(deps) if (kc == 0 and hf == 0) else []
                if kdeps is not None and hf == 0:
                    d_.append(kdeps(kc))
                tok = P.op(
                    "pe",
                    lambda e, kc=kc, hf=hf: e.matmul(
                        out=ps[:, hf * 512:(hf + 1) * 512],
                        lhsT=WB[slot][:, woff + kc * 128: woff + (kc + 1) * 128],
                        rhs=rhs_of(kc)[:, hf * 512:(hf + 1) * 512],
                        start=(kc == 0), stop=(kc == kcn - 1)),
                    deps=d_, ms=last)
        return tok

    con = lambda off, n=1: CON[:, off:off + n]

    ctok = P.op("sp", lambda e: e.dma_start(out=CON[:, :], in_=consts_d[:, :]), dsem=P.dma_sem("cld"))
    t_idb = P.op("dve", lambda e: e.tensor_copy(out=IDB[:, :], in_=con(c.c_ident, 128)), deps=[ctok])
    P.barrier()

    nld = [P.dma_sem(f"nld{i}") for i in range(3)]
    nst = [P.dma_sem(f"nst{i}") for i in range(2)]

    def stage_norm(xsrc, gidx, final=False, have_acc=False):
        ring = [FT[0], FT[1], FT[2]]
        SQ, ACC, RSTD = FT[3], FT[4], FT[5]
        free = [None] * 3
        ld = {}

        def load(i):
            kc = i % KC
            s_ = i % 3
            ld[i] = P.op("sp", lambda e, kc=kc, s_=s_: e.dma_start(out=ring[s_][:, :], in_=xsrc[kc, :, :]),
                         deps=[free[s_]], dsem=nld[s_])
        n2 = 2 * KC
        i0 = KC if have_acc else 0
        for i in range(i0, min(i0 + 2, n2)):
            load(i)
        acc_tok = None
        rstd_tok = None
        ofree = [None, None]
        if have_acc:
            tm = None
            for hf in range(NH):
                tm = P.op("pe", lambda e, hf=hf: e.matmul(
                    out=PS[0][:, hf * 512:(hf + 1) * 512], lhsT=con(c.c_onesd, 128),
                    rhs=ACC[:, hf * 512:(hf + 1) * 512], start=True, stop=True), ms=(hf == NH - 1))
            tsq_ = P.op("act", lambda e: e.activation(out=RSTD[:, :], in_=PS[0][:, :], func=AF.Sqrt,
                                                      bias=con(c.c_eps), scale=1.0), deps=[tm])
            rstd_tok = P.op("dve", lambda e: e.reciprocal(out=RSTD[:, :], in_=RSTD[:, :]), deps=[tsq_])
        for i in range(i0, n2):
            if i + 2 < n2:
                load(i + 2)
            kc = i % KC
            s_ = i % 3
            if i < KC:
                if kc == 0:
                    t1 = P.op("act", lambda e, s_=s_: e.activation(out=ACC[:, :], in_=ring[s_][:, :], func=AF.Square),
                              deps=[ld[i]])
                    acc_tok = t1
                    free[s_] = t1
                else:
                    t1 = P.op("act", lambda e, s_=s_: e.activation(out=SQ[:, :], in_=ring[s_][:, :], func=AF.Square),
                              deps=[ld[i], acc_tok])
                    free[s_] = t1
                    acc_tok = P.op("dve", lambda e: e.tensor_tensor(out=ACC[:, :], in0=ACC[:, :], in1=SQ[:, :], op=ALU.add),
                                   deps=[t1, acc_tok])
                if kc == KC - 1:
                    tm = None
                    for hf in range(NH):
                        tm = P.op("pe", lambda e, hf=hf: e.matmul(
                            out=PS[0][:, hf * 512:(hf + 1) * 512], lhsT=con(c.c_onesd, 128),
                            rhs=ACC[:, hf * 512:(hf + 1) * 512], start=True, stop=True),
                            deps=[acc_tok], ms=(hf == NH - 1))
                    tsq_ = P.op("act", lambda e: e.activation(out=RSTD[:, :], in_=PS[0][:, :], func=AF.Sqrt,
                                                              bias=con(c.c_eps), scale=1.0), deps=[tm])
                    rstd_tok = P.op("dve", lambda e: e.reciprocal(out=RSTD[:, :], in_=RSTD[:, :]), deps=[tsq_])
            else:
                gcol = c.c_norm + gidx * KC + kc
                if not final:
                    t2 = P.op("dve", lambda e, s_=s_, kc=kc, gcol=gcol: e.scalar_tensor_tensor(
                        out=ACTB[:, kc, :], in0=ring[s_][:, :], scalar=con(gcol), in1=RSTD[:, :],
                        op0=ALU.mult, op1=ALU.mult), deps=[ld[i], rstd_tok])
                    free[s_] = t2
                else:
                    o_ = kc % 2
                    OT = [FT[3], FT[4]][o_]
                    t2 = P.op("dve", lambda e, s_=s_, OT=OT, gcol=gcol: e.scalar_tensor_tensor(
                        out=OT[:, :], in0=ring[s_][:, :], scalar=con(gcol), in1=RSTD[:, :],
                        op0=ALU.mult, op1=ALU.mult), deps=[ld[i], rstd_tok, ofree[o_]])
                    free[s_] = t2
                    ofree[o_] = P.op("sp", lambda e, OT=OT, kc=kc: e.dma_start(out=y[kc, :, :], in_=OT[:, :]),
                                     deps=[t2], dsem=nst[o_])
        P.barrier()

    ast_sem = [P.dma_sem(f"ast{i}") for i in range(3)]

    def stage_gateup():
        SG = [FT[0], FT[1]]
        AST = [BT[0], BT[1], BT[2]]
        sgfree = [None, None]
        astfree = [None] * 3
        for fc in range(FC):
            slot, ltok = w_next()
            pg, pu = PS[(2 * fc) % 4], PS[(2 * fc + 1) % 4]
            ig, iu = (2 * fc) % 4, (2 * fc + 1) % 4
            tg = mm_group(pg, slot, 0, KC, lambda kc: ACTB[:, kc, :], [ltok, psfree[ig]])
            tu = mm_group(pu, slot, KC * 128, KC, lambda kc: ACTB[:, kc, :], [psfree[iu]])
            w_issue(tu)
            i_ = fc % 2
            j_ = fc % 3
            ta = P.op("act", lambda e, pg=pg, i_=i_: e.activation(out=SG[i_][:, :], in_=pg[:, :], func=AF.Silu),
                      deps=[tg, sgfree[i_]])
            psfree[ig] = ta
            tv = P.op("dve", lambda e, pu=pu, i_=i_, j_=j_: e.tensor_tensor(
                out=AST[j_][:, :], in0=pu[:, :], in1=SG[i_][:, :], op=ALU.mult), deps=[tu, ta, astfree[j_]])
            psfree[iu] = tv
            sgfree[i_] = tv
            astfree[j_] = P.op("sp", lambda e, fc=fc, j_=j_: e.dma_start(out=AT[fc, :, :], in_=AST[j_][:, :]),
                               deps=[tv], dsem=ast_sem[j_])
        P.barrier()
        for i in range(4):
            psfree[i] = None

    xld = [P.dma_sem(f"xld{i}") for i in range(3)]
    xst = [P.dma_sem(f"xst{i}") for i in range(3)]
    atl = [P.dma_sem(f"atl{i}") for i in range(4)]

    def stage_resid(xsrc, parts, scale, load_at):
        XT = [FT[0], FT[1], FT[2]]
        SQ, ACC = FT[3], FT[4]
        xfree = [None] * 3
        sqfree = [None] * 3
        acc_tok = [None]
        stok = {}
        fc0 = 0
        for pi, ps_ in enumerate(parts):
            atok = None
            ktok = {}
            if load_at:
                k0 = 0
                di = 0
                while k0 < ps_:
                    k1 = min(ps_, k0 + 8)
                    t_ = P.op("sp", lambda e, k0=k0, k1=k1, fc0=fc0: e.dma_start(
                        out=ACTB[:, k0:k1, :], in_=AT[fc0 + k0:fc0 + k1, :, :].rearrange("c p t -> p c t")),
                        dsem=atl[di % 4])
                    for kk in range(k0, k1):
                        ktok[kk] = t_
                    k0 = k1
                    di += 1
            src = xsrc if pi == 0 else XS
            ld = {}

            def load(dc):
                s_ = dc % 3
                ld[dc] = P.op("sp", lambda e, dc=dc, s_=s_, src=src: e.dma_start(out=XT[s_][:, :], in_=src[dc, :, :]),
                              deps=[xfree[s_], sqfree[s_], stok.get(dc)], dsem=xld[s_])
            for dc in range(min(2, KC)):
                load(dc)
            for dc in range(KC):
                if dc + 2 < KC:
                    load(dc + 2)
                slot, ltok = w_next()
                ip = dc % 4
                tm = mm_group(PS[ip], slot, 0, ps_, lambda kc: ACTB[:, kc, :], [ltok, psfree[ip], atok],
                              kdeps=((lambda kc: ktok.get(kc)) if (load_at and dc == 0) else None))
                w_issue(tm)
                s_ = dc % 3
                tv = P.op("dve", lambda e, ip=ip, s_=s_: e.scalar_tensor_tensor(
                    out=XT[s_][:, :], in0=PS[ip][:, :], scalar=float(scale), in1=XT[s_][:, :],
                    op0=ALU.mult, op1=ALU.add), deps=[tm, ld[dc]])
                psfree[ip] = tv
                stok[dc] = P.op("sp", lambda e, dc=dc, s_=s_: e.dma_start(out=XS[dc, :, :], in_=XT[s_][:, :]),
                                deps=[tv], dsem=xst[s_])
                xfree[s_] = stok[dc]
                if pi == len(parts) - 1:
                    if dc == 0:
                        t1 = P.op("act", lambda e, s_=s_: e.activation(out=ACC[:, :], in_=XT[s_][:, :], func=AF.Square),
                                  deps=[tv])
                        acc_tok[0] = t1
                    else:
                        t1 = P.op("act", lambda e, s_=s_: e.activation(out=SQ[:, :], in_=XT[s_][:, :], func=AF.Square),
                                  deps=[tv, acc_tok[0]])
                        acc_tok[0] = P.op("dve", lambda e: e.tensor_tensor(out=ACC[:, :], in0=ACC[:, :], in1=SQ[:, :],
                                                                           op=ALU.add), deps=[t1, acc_tok[0]])
                    sqfree[s_] = t1
            fc0 += ps_
            P.barrier()
            for i in range(4):
                psfree[i] = None
            for i in range(3):
                xfree[i] = None

    pst = [P.dma_sem(f"pst{i}") for i in range(2)]
    ml = [P.dma_sem(f"ml{i}") for i in range(8)]
    mst = [P.dma_sem(f"mst{i}") for i in range(4)]
    exs = P.dma_sem("exs")
    exl = P.dma_sem("exl")
    cvl = P.dma_sem("cvl")
    ccs = [es.enter_context(nc.semaphore(f"cc{l}")) for l in range(DEPTH)]
    L0, L1, T0, T1, T2, T3, T4 = FT
    KB, VB, KZ, VT, QB, QX, ST = BT
    gam = [1.0 - 2.0 ** (-5 - h) for h in range(H)]
    g128 = [float(np.float64(g) ** 128) for g in gam]

    def blk(t, n):
        return t[:, n * 128:(n + 1) * 128]

    RES = {}

    def _res(k):
        if k not in RES:
            RES[k] = [None, []]
        return RES[k]

    def _deps(reads, writes, extra=()):
        deps = list(extra)
        for k in reads:
            deps.append(_res(k)[0])
        for k in writes:
            r = _res(k)
            deps.append(r[0])
            deps += r[1]
        return deps

    def _upd(tok, reads, writes):
        if tok is None:
            return
        for k in reads:
            _res(k)[1].append(tok)
        for k in writes:
            r = _res(k)
            r[0] = tok
            r[1] = []

    def auto(eng, fn, reads=(), writes=(), dsem=None, extra=()):
        tok = P.op(eng, fn, _deps(reads, writes, extra), ms=(None if dsem is None else False), dsem=dsem)
        _upd(tok, reads, writes)
        return tok

    def auto_group(fns, reads=(), writes=(), extra=()):
        deps = _deps(reads, writes, extra)
        tok = None
        if c.pedrain:
            P.op("pe", lambda e: e.drain(), deps, ms=False)
            deps = ()
        for i, fn in enumerate(fns):
            tok = P.op("pe", fn, deps if i == 0 else (), ms=(i == len(fns) - 1))
        if c.pedrain:
            P.op("pe", lambda e: e.drain(), (), ms=False)
        _upd(tok, reads, writes)
        return tok

    def res_reset():
        RES.clear()

    order_in = ([H + h for h in range(H)] + [2 * H + h for h in range(H)] + [h for h in range(H)] +
                [5 * H + j for j in range(H)] + [6 * H + j for j in range(H)] + [4 * H + j for j in range(H)] +
                [3 * H + h for h in range(H)])

    def rope_bg(src, dst, psname, ps):
        auto_group([lambda e, hf=hf: e.matmul(out=ps[:, hf * 512:(hf + 1) * 512], lhsT=con(c.c_perm, 128),
                                              rhs=src[1][:, hf * 512:(hf + 1) * 512], start=True, stop=True)
                    for hf in range(NH)], reads=[src[0]], writes=[psname])
        yield
        auto("dve", lambda e: e.tensor_tensor(out=T0[:, :], in0=src[1][:, :], in1=con(c.c_cos, TT), op=ALU.mult),
             reads=[src[0]], writes=["T0"])
        auto("dve", lambda e: e.tensor_tensor(out=T1[:, :], in0=ps[:, :], in1=con(c.c_sin, TT), op=ALU.mult),
             reads=[psname], writes=["T1"])
        auto("dve", lambda e: e.tensor_tensor(out=dst[1][:, :], in0=T0[:, :], in1=T1[:, :], op=ALU.add),
             reads=["T0", "T1"], writes=[dst[0]])

    def bg_job(l, pt_ready):
        pk = PS[1][:, 0:TT // 2].bitcast(BF16)
        pv = PS[1][:, TT // 2:TT].bitcast(BF16)
        alldone = lambda: all(pt_ready(cc) for cc in range(NP))
        kv_loaded = set()

        def load_kv(h):
            kv_loaded.add(h)
            auto("sp", lambda e, h=h: e.dma_start(out=L0[:, :], in_=PT[H + h, :, :]), reads=[("pt", H + h)],
                 writes=["L0"], dsem=ml[0])
            auto("sp", lambda e, h=h: e.dma_start(out=L1[:, :], in_=PT[2 * H + h, :, :]), reads=[("pt", 2 * H + h)],
                 writes=["L1"], dsem=ml[1])

        for h in range(H):
            while c.gate in (1, 4) and h >= (1 if c.gate == 1 else 0) and not alldone():
                yield
            while not (pt_ready(H + h) and pt_ready(2 * H + h)):
                yield
            if h not in kv_loaded:
                load_kv(h)
            yield from rope_bg(("L0", L0), ("T2", T2), "PS0", PS[0])
            q_early = pt_ready(h)
            if q_early:
                auto("sp", lambda e, h=h: e.dma_start(out=L0[:, :], in_=PT[h, :, :]), reads=[("pt", h)], writes=["L0"],
                     dsem=ml[0])
            auto("act", lambda e: e.activation(out=KB[:, :], in_=T2[:, :], func=AF.Copy), reads=["T2"], writes=["KB"])
            auto("act", lambda e: e.activation(out=VB[:, :], in_=L1[:, :], func=AF.Copy), reads=["L1"], writes=["VB"])
            yield
            if not (c.bgskip & 8 and h >= 1):
                auto_group([lambda e, n=n: e.transpose(out=blk(pk, n), in_=blk(KB, n), identity=IDB[:, :]) for n in range(NCH)] +
                           [lambda e, n=n: e.transpose(out=blk(pv, n), in_=blk(VB, n), identity=IDB[:, :]) for n in range(NCH)],
                           reads=["KB", "VB"], writes=["PS1"])
            yield
            if not (c.bgskip & 1 and h >= 1):
                auto("dve", lambda e, h=h: e.tensor_scalar(out=KZ[:, :], in0=pk, scalar1=con(c.c_zeta + h), scalar2=None,
                                                           op0=ALU.mult), reads=["PS1"], writes=["KZ"])
            if not (c.bgskip & 2 and h >= 1):
                auto("act", lambda e: e.activation(out=VT[:, :], in_=pv, func=AF.Copy), reads=["PS1"], writes=["VT"])
            if c.kvbar and h >= 1:
                P.barrier()
            if not (c.bgskip & 4 and h >= 1):
                kvp = PS[c.kvps] if h >= 1 else PS[0]
                auto_group([lambda e, n=n, kvp=kvp: e.matmul(out=blk(kvp, n), lhsT=blk(KZ, n), rhs=blk(VT, n), start=True, stop=True)
                            for n in range(NCH)], reads=["KZ", "VT"], writes=["PS0"])
            yield
            while c.gate == 3 and h >= 1 and not alldone():
                yield
            auto("act", lambda e: e.activation(out=T2[:, :], in_=PS[0][:, :], func=AF.Copy), reads=["PS0"], writes=["T2"])
            auto("sp", lambda e, h=h: e.dma_start(out=KVS[h, :, :], in_=T2[:, :]), reads=["T2"], writes=[("kvs", h)],
                 dsem=mst[0])
            auto("dve", lambda e, h=h: e.tensor_copy(out=blk(SIN, h), in_=blk(T2, 0)), reads=["T2"], writes=["SIN"])
            for n in range(1, NCH):
                auto("dve", lambda e, h=h, n=n: e.scalar_tensor_tensor(
                    out=blk(SIN, h), in0=blk(SIN, h), scalar=g128[h], in1=blk(T2, n),
                    op0=ALU.mult, op1=ALU.add), reads=["T2", "SIN"], writes=["SIN"])
            while c.gate == 2 and h >= 1 and not alldone():
                yield
            if not q_early:
                while not pt_ready(h):
                    yield
                auto("sp", lambda e, h=h: e.dma_start(out=L0[:, :], in_=PT[h, :, :]), reads=[("pt", h)], writes=["L0"],
                     dsem=ml[0])
            yield from rope_bg(("L0", L0), ("L1", L1), "PS0", PS[0])
            auto("act", lambda e: e.activation(out=QB[:, :], in_=L1[:, :], func=AF.Copy), reads=["L1"], writes=["QB"])
            auto("dve", lambda e, h=h: e.tensor_tensor(
                out=QX[:, :].rearrange("p (n c) -> p n c", c=128), in0=L1[:, :].rearrange("p (n c) -> p n c", c=128),
                in1=con(c.c_xi + h * 128, 128).unsqueeze(1).to_broadcast([128, NCH, 128]), op=ALU.mult),
                 reads=["L1"], writes=["QX"])
            auto("sp", lambda e, h=h: e.dma_start(out=QXS[h, :, :], in_=QX[:, :]), reads=["QX"], writes=[("qxs", h)],
                 dsem=mst[1])
            auto_group([lambda e, n=n: e.matmul(out=blk(PS[1], n), lhsT=blk(KB, n), rhs=blk(QB, n), start=True, stop=True)
                        for n in range(NCH)], reads=["KB", "QB"], writes=["PS1"])
            if h + 1 < H and pt_ready(H + h + 1) and pt_ready(2 * H + h + 1):
                load_kv(h + 1)
            yield
            auto("dve", lambda e, h=h: e.tensor_tensor(
                out=ST[:, :].rearrange("p (n c) -> p n c", c=128), in0=PS[1][:, :].rearrange("p (n c) -> p n c", c=128),
                in1=con(c.c_dm + h * 128, 128).unsqueeze(1).to_broadcast([128, NCH, 128]), op=ALU.mult),
                 reads=["PS1"], writes=["ST"])
            auto_group([lambda e, n=n: e.matmul(out=blk(PS[0], n), lhsT=blk(VT, n), rhs=blk(ST, n), start=True, stop=True)
                        for n in range(NCH)], reads=["VT", "ST"], writes=["PS0"])
            yield
            auto("act", lambda e: e.activation(out=T3[:, :], in_=PS[0][:, :], func=AF.Copy), reads=["PS0"], writes=["T3"])
            auto("sp", lambda e, h=h: e.dma_start(out=INTRA[h, :, :], in_=T3[:, :]), reads=["T3"], writes=[("intra", h)],
                 dsem=mst[2])
        while c.convlate and not all(pt_ready(cc) for cc in range(NP)):
            yield
        auto("dve", lambda e: e.memset(CU[:, 0:2], 0.0), writes=["CU"])
        for j in range(H):
            while not (pt_ready(4 * H + j) and pt_ready(5 * H + j) and pt_ready(6 * H + j)):
                yield
            auto("sp", lambda e, j=j: e.dma_start(out=L0[:, :], in_=PT[5 * H + j, :, :]), reads=[("pt", 5 * H + j)],
                 writes=["L0"], dsem=ml[0])
            auto("sp", lambda e, j=j: e.dma_start(out=L1[:, :], in_=PT[6 * H + j, :, :]), reads=[("pt", 6 * H + j)],
                 writes=["L1"], dsem=ml[1])
            auto("sp", lambda e, j=j: e.dma_start(out=T2[:, :], in_=PT[4 * H + j, :, :]), reads=[("pt", 4 * H + j)],
                 writes=["T2"], dsem=ml[2])
            auto("dve", lambda e: e.tensor_tensor(out=CU[:, 2:TT + 2], in0=L0[:, :], in1=L1[:, :], op=ALU.mult),
                 reads=["L0", "L1"], writes=["CU"])
            cw = lambda tap, j=j: con(c.c_cw + (l * 3 + tap) * H + j)
            auto("dve", lambda e, j=j: e.tensor_copy(out=HALO[:, j, :], in_=CU[:, TT:TT + 2]), reads=["CU"], writes=["HALO"])
            auto("dve", lambda e, cw=cw: e.tensor_scalar(out=T0[:, :], in0=CU[:, 2:TT + 2], scalar1=cw(2), scalar2=None,
                                                        op0=ALU.mult), reads=["CU"], writes=["T0"])
            auto("dve", lambda e, cw=cw: e.scalar_tensor_tensor(out=T0[:, :], in0=CU[:, 1:TT + 1], scalar=cw(1),
                                                               in1=T0[:, :], op0=ALU.mult, op1=ALU.add),
                 reads=["CU", "T0"], writes=["T0"])
            auto("dve", lambda e, cw=cw: e.scalar_tensor_tensor(out=T0[:, :], in0=CU[:, 0:TT], scalar=cw(0),
                                                               in1=T0[:, :], op0=ALU.mult, op1=ALU.add),
                 reads=["CU", "T0"], writes=["T0"])
            auto("dve", lambda e, j=j: e.tensor_copy(out=Y2[:, j, :], in_=T0[:, 0:2]), reads=["T0"], writes=["Y2"])
            auto("dve", lambda e, j=j: e.tensor_copy(out=B2[:, j, :], in_=T2[:, 0:2]), reads=["T2"], writes=["B2"])
            auto("dve", lambda e: e.tensor_tensor(out=KB[:, :], in0=T0[:, :], in1=T2[:, :], op=ALU.mult),
                 reads=["T0", "T2"], writes=["KB"])
            auto("sp", lambda e, j=j: e.dma_start(out=CVO[j, :, :], in_=KB[:, :]), reads=["KB"], writes=[("cvo", j)],
                 dsem=mst[3])
            yield

    def stage_mix(l):
        res_reset()
        ptok = {}
        ready = lambda cc: cc in ptok
        bg = bg_job(l, ready)
        last_tm = [None]
        st_ = {"alive": not c.nobg}

        def adv():
            st_["n"] = st_.get("n", 0) + 1
            if st_["n"] > c.bgsteps:
                st_["alive"] = False
            if st_["alive"]:
                try:
                    next(bg)
                except StopIteration:
                    st_["alive"] = False

        for i, cc in enumerate(order_in):
            slot, ltok = w_next()
            ip = 2 + (i % 2)
            psn = f"PS{ip}"
            deps = _deps([], [psn], [ltok])
            tm = mm_group(PS[ip], slot, 0, KC, lambda kc: ACTB[:, kc, :], deps, mid=(adv if (i >= 2 * H and c.bg2) else None))
            _upd(tm, [], [psn])
            last_tm[0] = tm
            w_issue(tm)
            if i % 2 == 0:
                auto("act", lambda e, ip=ip: e.activation(out=T4[:, :], in_=PS[ip][:, :], func=AF.Copy),
                     reads=[psn], writes=["T4"])
            else:
                auto("dve", lambda e, ip=ip: e.tensor_copy(out=T4[:, :], in_=PS[ip][:, :]), reads=[psn], writes=["T4"])
            ptok[cc] = auto("sp", lambda e, cc=cc: e.dma_start(out=PT[cc, :, :], in_=T4[:, :]), reads=["T4"],
                            writes=[("pt", cc)], dsem=pst[i % 2])
            if i >= 2 * H - 1:
                for _ in range(c.bgn):
                    adv()
        while st_["alive"]:
            adv()
        if c.stopat == 1:
            raise StopBuild()
        auto("sp", lambda e: e.dma_start(out=ACTB[:, H:2 * H, :], in_=CVO[:, :, :].rearrange("c p t -> p c t")),
             reads=[("cvo", j) for j in range(H)], writes=["ACTBc"], dsem=cvl, extra=[last_tm[0]])
        auto("dve", lambda e: e.tensor_copy(out=SIN[:, H * 128:c.EW].rearrange("p (c t) -> p c t", t=2), in_=HALO[:, :, :]),
             reads=["HALO", "SIN"], writes=["SIN"])
        ts = auto("sp", lambda e: e.dma_start(out=EXI[l][:, :], in_=SIN[:, :]), reads=["SIN"], writes=["EXI"], dsem=exs)
        P.op("pool", lambda e: e.collective_compute(
            "AllGather", ALU.bypass, replica_groups=[[2 * i, 2 * i + 1] for i in range(NCORES // 2)],
            ins=[EXI[l].ap().opt()], outs=[EXO[l].ap().opt()]).then_inc(ccs[l]), deps=[ts], ms=False)
        P.op("pool", lambda e: e.wait_ge(ccs[l], 1), ms=False)
        tl = P.op("pool", lambda e: e.dma_start(out=SIN[:, :], in_=EXO[l][0:128, :]), dsem=exl)
        P.barrier()
        res_reset()
        auto("dve", lambda e: e.tensor_scalar(out=SIN[:, :], in0=SIN[:, :], scalar1=con(c.c_sel), scalar2=None,
                                              op0=ALU.mult), writes=["SIN"], extra=[tl])
        if c.stopat == 2:
            raise StopBuild()
        HL = SIN[:, H * 128:c.EW].rearrange("p (c t) -> p c t", t=2)
        W0 = con(c.c_cw + (l * 3 + 0) * H, H)
        W1 = con(c.c_cw + (l * 3 + 1) * H, H)
        auto("dve", lambda e: e.tensor_tensor(out=FX[:, :, 0], in0=HL[:, :, 1], in1=W1, op=ALU.mult), reads=["SIN"], writes=["FX"])
        auto("dve", lambda e: e.tensor_tensor(out=FX[:, :, 1], in0=HL[:, :, 0], in1=W0, op=ALU.mult), reads=["SIN"], writes=["FX"])
        auto("dve", lambda e: e.tensor_tensor(out=FX[:, :, 0], in0=FX[:, :, 0], in1=FX[:, :, 1], op=ALU.add), reads=["FX"], writes=["FX"])
        auto("dve", lambda e: e.tensor_tensor(out=FX[:, :, 1], in0=HL[:, :, 1], in1=W0, op=ALU.mult), reads=["SIN"], writes=["FX"])
        auto("dve", lambda e: e.tensor_tensor(out=Y2[:, :, :], in0=Y2[:, :, :], in1=FX[:, :, :], op=ALU.add), reads=["FX"], writes=["Y2"])
        auto("dve", lambda e: e.tensor_tensor(out=ACTB[:, H:2 * H, 0:2], in0=Y2[:, :, :], in1=B2[:, :, :], op=ALU.mult),
             reads=["Y2", "ACTBc"], writes=["ACTBc"])
        if c.stopat == 3:
            raise StopBuild()
        sets = [dict(I=("L0", L0), K=("L1", L1), G=("T0", T0), Q=("KB", KB), ps=("PS0", PS[0]), pn=("PS2", PS[2])),
                dict(I=("T1", T1), K=("T2", T2), G=("T3", T3), Q=("VB", VB), ps=("PS1", PS[1]), pn=("PS3", PS[3]))]

        def loads_kq(h):
            s_ = sets[h % 2]
            o = 4 * (h % 2)
            auto("sp", lambda e: e.dma_start(out=s_["K"][1][:, :], in_=KVS[h, :, :]), writes=[s_["K"][0]], dsem=ml[o + 1])
            auto("sp", lambda e: e.dma_start(out=s_["Q"][1][:, :], in_=QXS[h, :, :]), writes=[s_["Q"][0]], dsem=ml[o + 2])

        def loads_ig(h):
            s_ = sets[h % 2]
            o = 4 * (h % 2)
            auto("sp", lambda e: e.dma_start(out=s_["I"][1][:, :], in_=INTRA[h, :, :]), writes=[s_["I"][0]], dsem=ml[o + 0])
            auto("sp", lambda e: e.dma_start(out=s_["G"][1][:, :], in_=PT[3 * H + h, :, :]), writes=[s_["G"][0]], dsem=ml[o + 3])

        def part1(h):
            s_ = sets[h % 2]
            Kn, Kt = s_["K"]
            auto("dve", lambda e: e.tensor_copy(out=blk(SST, 0), in_=blk(SIN, h)), reads=["SIN"], writes=["SST"])
            for n in range(NCH):
                auto("dve", lambda e, n=n: e.scalar_tensor_tensor(
                    out=blk(SST, n + 1), in0=blk(SST, n), scalar=g128[h], in1=blk(Kt, n),
                    op0=ALU.mult, op1=ALU.add), reads=["SST", Kn], writes=["SST"])
            auto("act", lambda e: e.activation(out=SBALL[:, :], in_=SST[:, 0:NCH * 128], func=AF.Copy),
                 reads=["SST"], writes=["SBALL"])
            psn, ps = s_["ps"]
            Qn, Qt = s_["Q"]
            auto_group([lambda e, n=n: e.matmul(out=blk(ps, n), lhsT=blk(SBALL, n), rhs=blk(Qt, n), start=True, stop=True)
                        for n in range(NCH)], reads=["SBALL", Qn], writes=[psn])

        def part2(h):
            s_ = sets[h % 2]
            In, It = s_["I"]
            Gn, Gt = s_["G"]
            psn, ps = s_["ps"]
            pnn, pn = s_["pn"]
            auto("dve", lambda e: e.tensor_tensor(out=It[:, :], in0=ps[:, :], in1=It[:, :], op=ALU.add),
                 reads=[psn, In], writes=[In])
            auto("act", lambda e: e.activation(out=T4[:, :], in_=It[:, :], func=AF.Square), reads=[In], writes=["T4"])
            auto_group([lambda e, hf=hf: e.matmul(out=pn[:, hf * 512:(hf + 1) * 512], lhsT=con(c.c_onesh, 128),
                                                  rhs=T4[:, hf * 512:(hf + 1) * 512], start=True, stop=True)
                        for hf in range(NH)], reads=["T4"], writes=[pnn])
            auto("act", lambda e: e.activation(out=T4[:, :], in_=pn[:, :], func=AF.Sqrt, bias=con(c.c_eps), scale=1.0),
                 reads=[pnn], writes=["T4"])
            auto("act", lambda e: e.activation(out=Gt[:, :], in_=Gt[:, :], func=AF.Silu), reads=[Gn], writes=[Gn])
            auto("dve", lambda e: e.reciprocal(out=T4[:, :], in_=T4[:, :]), reads=["T4"], writes=["T4"])
            auto("dve", lambda e: e.tensor_tensor(out=It[:, :], in0=It[:, :], in1=T4[:, :], op=ALU.mult),
                 reads=[In, "T4"], writes=[In])
            auto("dve", lambda e: e.scalar_tensor_tensor(
                out=ACTB[:, h, :], in0=It[:, :], scalar=con(c.c_rn + l * H + h), in1=Gt[:, :],
                op0=ALU.mult, op1=ALU.mult), reads=[In, Gn], writes=[("actb", h)])

        loads_kq(0)
        loads_ig(0)
        for h in range(H + 1):
            if h + 1 < H:
                loads_kq(h + 1)
            if h < H:
                part1(h)
            if h >= 1:
                part2(h - 1)
            if h + 1 < H:
                loads_ig(h + 1)
            if c.pbser:
                P.barrier()
        P.barrier()
        res_reset()

    dbs = P.dma_sem("dbs")

    def dump_r():
        P.op("sp", lambda e: e.dma_start(out=DBR[:, :, :].rearrange("c p t -> p c t"), in_=ACTB[:, :, :]), dsem=dbs)
        P.barrier()

    xcur = xin
    try:
      for l in range(DEPTH):
          stage_norm(xcur, 3 * l + 0, have_acc=(l > 0))
          stage_gateup()
          stage_resid(xcur, c.parts, 0.5, True)
          xcur = XS
          if c.mixer:
              stage_norm(xcur, 3 * l + 1, have_acc=True)
              stage_mix(l)
              if c.debug and l == 0:
                  dump_r()
              stage_resid(xcur, [KC], 1.0, False)
          stage_norm(xcur, 3 * l + 2, have_acc=c.mixer)
          stage_gateup()
          stage_resid(xcur, c.parts, 0.5, True)
      stage_norm(xcur, 3 * DEPTH, final=True, have_acc=True)
      assert wstate["consumed"] == len(wchunks), (wstate, len(wchunks))
    except StopBuild:
      P.barrier()

    assert P.simulate(), "deadlock in program order"
    with nc.Block() as block:
        P.emit(block)
    es.close()
    return nc


def _consts(cfg, half, norms, ret_norm, conv_w):
    c = cfg
    H, TT = c.H, c.TT
    A = np.zeros((128, c.CW), np.float32)
    A[:, c.c_ident:c.c_ident + 128] = np.eye(128, dtype=np.float32)
    perm = np.zeros((128, 128), np.float32)
    for dp in range(128):
        perm[(dp + 64) % 128, dp] = 1.0
    A[:, c.c_perm:c.c_perm + 128] = perm
    A[:, c.c_onesd:c.c_onesd + 128] = 1.0 / c.D
    A[:, c.c_onesh:c.c_onesh + 128] = 1.0 / 128.0
    half_d = 64
    inv_freq = (10000.0 ** (-np.arange(half_d, dtype=np.float32) / np.float32(half_d))).astype(np.float32)
    pos = (half * TT + np.arange(TT)).astype(np.float32)
    ang = (pos[None, :] * inv_freq[:, None]).astype(np.float32)
    cos = np.cos(ang).astype(np.float32)
    sin = np.sin(ang).astype(np.float32)
    A[:, c.c_cos:c.c_cos + TT] = np.concatenate([cos, cos], 0)
    A[:, c.c_sin:c.c_sin + TT] = np.concatenate([-sin, sin], 0)
    hh = np.arange(H, dtype=np.float64)
    lg = np.log1p(-np.power(2.0, -5.0 - hh))
    idx = np.arange(128, dtype=np.float64)
    scale = 128.0 ** -0.5
    diff = idx[None, :] - idx[:, None]
    dm = np.where(diff[None] >= 0, np.exp(lg[:, None, None] * np.maximum(diff, 0.0)[None]), 0.0) * scale
    A[:, c.c_dm:c.c_dm + H * 128] = dm.transpose(1, 0, 2).reshape(128, H * 128)
    xi = np.exp(lg[:, None] * (idx + 1.0)[None, :])
    A[:, c.c_xi:c.c_xi + H * 128] = np.broadcast_to(xi.reshape(1, H * 128), (128, H * 128))
    zeta = np.exp(lg[:, None] * (127.0 - idx)[None, :]) * scale
    A[:, c.c_zeta:c.c_zeta + H] = zeta.T
    A[:, c.c_sel] = float(half)
    A[:, c.c_eps] = EPS
    for i, g in enumerate(norms):
        A[:, c.c_norm + i * c.KC:c.c_norm + (i + 1) * c.KC] = g.reshape(c.KC, 128).T
    for l in range(c.DEPTH):
        A[:, c.c_rn + l * H:c.c_rn + (l + 1) * H] = ret_norm[l].reshape(H, 128).T
        for tap in range(3):
            o = c.c_cw + (l * 3 + tap) * H
            A[:, o:o + H] = conv_w[l, tap].reshape(H, 128).T
    return A


def _lay_cols(w, KC):
    K, N = w.shape
    return np.ascontiguousarray(w.reshape(KC, 128, N // 128, 128).transpose(2, 1, 0, 3)).reshape(N // 128, 128, KC * 128)


def prepare_inputs(cfg, x, norm_ffa, w_ffa_gate, w_ffa_up, w_ffa_down, norm_mix, w_in, conv_w, ret_norm, w_out,
                   norm_ffb, w_ffb_gate, w_ffb_up, w_ffb_down, norm_final):
    c = cfg
    KC, FC, TT = c.KC, c.FC, c.TT
    shared = {}
    for l in range(c.DEPTH):
        for f, (wg, wu, wdn) in (("a", (w_ffa_gate, w_ffa_up, w_ffa_down)), ("b", (w_ffb_gate, w_ffb_up, w_ffb_down))):
            g = _lay_cols(np.asarray(wg[l]), KC).reshape(FC, 128, 1, KC * 128)
            u = _lay_cols(np.asarray(wu[l]), KC).reshape(FC, 128, 1, KC * 128)
            shared[f"wgu_{l}{f}"] = np.concatenate([g, u], axis=2).reshape(FC, 128, 2 * KC * 128)
            wdl = np.zeros((3 * KC, 128, c.PSMAX * 128), np.float32)
            r0 = 0
            wdn_l = np.asarray(wdn[l])
            for pi, ps_ in enumerate(c.parts):
                blk = wdn_l[r0 * 128:(r0 + ps_) * 128]
                wdl[pi * KC:(pi + 1) * KC, :, 0:ps_ * 128] = _lay_cols(blk, ps_)
                r0 += ps_
            shared[f"wd_{l}{f}"] = wdl
        shared[f"win_{l}"] = _lay_cols(np.asarray(w_in[l]), KC)
        shared[f"wout_{l}"] = _lay_cols(np.asarray(w_out[l]), KC)
    norms = []
    for l in range(c.DEPTH):
        norms += [np.asarray(norm_ffa[l]), np.asarray(norm_mix[l]), np.asarray(norm_ffb[l])]
    norms.append(np.asarray(norm_final))
    x = np.asarray(x)
    in_maps = []
    for core in range(NCORES):
        b, half = core // 2, core % 2
        xt = np.ascontiguousarray(x[b, half * TT:(half + 1) * TT, :].T).reshape(KC, 128, TT)
        m = dict(shared)
        m["xin"] = xt
        m["consts"] = _consts(c, half, norms, np.asarray(ret_norm), np.asarray(conv_w))
        in_maps.append(m)
    return in_maps


def run(cfg, inputs, trace=False):
    nc = build_nc(cfg)
    in_maps = prepare_inputs(cfg, **inputs)
    res = run_bass_kernel_spmd(nc, in_maps, core_ids=list(range(NCORES)), **({"trace": True} if trace else {}))
    B = NCORES // 2
    out = np.zeros((B, 2 * cfg.TT, cfg.D), np.float32)
    for core in range(NCORES):
        b, half = core // 2, core % 2
        yv = np.asarray(res.results[core]["y"]).reshape(cfg.D, cfg.TT)
        out[b, half * cfg.TT:(half + 1) * cfg.TT, :] = yv.T
    return out, res


def kernel(**inputs):
    cfg = Cfg()
    out, _ = run(cfg, inputs)
    return out
```

```python
import numpy as np
from contextlib import ExitStack
import concourse.bass as bass
import concourse.mybir as mybir
from concourse.bass_utils import run_bass_kernel_spmd

F32 = mybir.dt.float32
BF16 = mybir.dt.bfloat16
ALU = mybir.AluOpType
AF = mybir.ActivationFunctionType
EPS = 1e-6
NCORES = 8


class StopBuild(Exception):
    pass


class Cfg:
    def __init__(s, D=4096, FF=11008, DEPTH=2, TT=1024, mixer=True, debug=False):
        s.debug = debug
        import os
        s.bg2 = bool(int(os.environ.get('BG2', '1')))
        s.bgn = int(os.environ.get('BGN', '1'))
        s.gate = int(os.environ.get('BGGATE', '0'))
        s.pbser = int(os.environ.get('PBSER', '0'))
        s.stopat = int(os.environ.get('STOPAT', '0'))
        s.nobg = int(os.environ.get('NOBG', '0'))
        s.pedrain = int(os.environ.get('PEDRAIN', '0'))
        s.bgsteps = int(os.environ.get('BGSTEPS', '100000'))
        s.bgskip = int(os.environ.get('BGSKIP', '0'))
        s.kvbar = int(os.environ.get('KVBAR', '0'))
        s.kvps = int(os.environ.get('KVPS', '0'))
        s.convlate = bool(int(os.environ.get('CONVLATE', '0')))
        s.D, s.FF, s.DEPTH, s.TT = D, FF, DEPTH, TT
        s.KC = D // 128
        s.FC = FF // 128
        s.H = D // 256
        s.NP = 7 * s.H
        s.NCH = TT // 128
        s.NH = TT // 512
        npart = 3
        base, rem = divmod(s.FC, npart)
        s.parts = [base + (1 if i < rem else 0) for i in range(npart)]
        s.PSMAX = max(s.parts)
        s.mixer = mixer
        s.EW = s.H * 128 + 2 * s.H
        o = 0
        def take(n):
            nonlocal o
            r = o
            o += n
            return r
        s.c_ident = take(128)
        s.c_perm = take(128)
        s.c_onesd = take(128)
        s.c_onesh = take(128)
        s.c_cos = take(TT)
        s.c_sin = take(TT)
        s.c_dm = take(s.H * 128)
        s.c_xi = take(s.H * 128)
        s.c_zeta = take(s.H)
        s.c_sel = take(1)
        s.c_eps = take(1)
        s.c_norm = take((3 * DEPTH + 1) * s.KC)
        s.c_rn = take(DEPTH * s.H)
        s.c_cw = take(DEPTH * 3 * s.H)
        s.CW = o
        s.WSLOT = max(2 * s.KC * 128, s.PSMAX * 128)


class Prog:
    ENG = ("pe", "act", "dve", "pool", "sp")

    def __init__(s, nc, es):
        s.nc, s.es = nc, es
        s.ops = {e: [] for e in s.ENG}
        s.sem = {e: es.enter_context(nc.semaphore("ms_" + e)) for e in ("pe", "act", "dve")}
        s.cnt = {e: 0 for e in s.ENG}
        s.waited = {e: {} for e in s.ENG}
        s.dsem, s.dcnt = {}, {}
        s.lastop = {e: None for e in s.ENG}
        s.pending_dma = []

    def dma_sem(s, name):
        s.dsem[name] = s.es.enter_context(s.nc.semaphore(name))
        s.dcnt[name] = 0
        return name

    def _wait(s, eng, tok):
        if tok is None:
            return
        kind, key, val = tok
        if eng == "pe" and kind == "ms" and key == "pe":
            return
        w = s.waited[eng]
        if w.get((kind, key), 0) >= val:
            return
        w[(kind, key)] = val
        s.ops[eng].append(["wait", kind, key, val])

    def _wait_all(s, eng, deps):
        best = {}
        for d in deps:
            for t in (d if isinstance(d, list) else [d]):
                if t is None:
                    continue
                k = (t[0], t[1])
                if k not in best or best[k][2] < t[2]:
                    best[k] = t
        for t in best.values():
            s._wait(eng, t)

    def op(s, eng, fn, deps=(), ms=None, dsem=None, track=True):
        if ms is None:
            ms = eng in ("act", "dve")
        s._wait_all(eng, deps)
        rec = ["op", fn, None]
        tok = None
        if dsem is not None:
            s.dcnt[dsem] += 16
            tok = ("dma", dsem, s.dcnt[dsem])
            rec[2] = ("dma", dsem)
            if track:
                s.pending_dma.append(tok)
        elif ms:
            s.cnt[eng] += 1
            tok = ("ms", eng, s.cnt[eng])
            rec[2] = ("ms", eng)
        s.ops[eng].append(rec)
        s.lastop[eng] = (rec, tok)
        return tok

    def last_tok(s, eng):
        lo = s.lastop[eng]
        if lo is None:
            return None
        rec, tok = lo
        if tok is None and rec[2] is None:
            s.cnt[eng] += 1
            tok = ("ms", eng, s.cnt[eng])
            rec[2] = ("ms", eng)
            s.lastop[eng] = (rec, tok)
        return tok

    def barrier(s):
        toks = [s.last_tok(e) for e in ("pe", "act", "dve")] + s.pending_dma
        s.pending_dma = []
        for e in ("pe", "act", "dve", "sp"):
            s._wait_all(e, [t for t in toks if t is not None and not (t[0] == "ms" and t[1] == e and e == "pe")])

    def simulate(s):
        val = {}
        pos = {e: 0 for e in s.ENG}
        while True:
            prog = False
            for e in s.ENG:
                lst = s.ops[e]
                while pos[e] < len(lst):
                    rec = lst[pos[e]]
                    if rec[0] == "wait":
                        if val.get((rec[1], rec[2]), 0) >= rec[3]:
                            pos[e] += 1
                            prog = True
                        else:
                            break
                    else:
                        if rec[2] is not None:
                            val[rec[2]] = val.get(rec[2], 0) + (1 if rec[2][0] == "ms" else 16)
                        pos[e] += 1
                        prog = True
            if all(pos[e] == len(s.ops[e]) for e in s.ENG):
                return True
            if not prog:
                for e in s.ENG:
                    if pos[e] < len(s.ops[e]):
                        print("DEADLOCK", e, "at", pos[e], "/", len(s.ops[e]), s.ops[e][pos[e]][:4],
                              "cur", val.get((s.ops[e][pos[e]][1], s.ops[e][pos[e]][2])) if s.ops[e][pos[e]][0] == "wait" else "")
                return False

    def emit(s, block):
        def replay(eng, lst):
            def body(e):
                for rec in lst:
                    if rec[0] == "wait":
                        _, kind, key, val = rec
                        sem = s.sem[key] if kind == "ms" else s.dsem[key]
                        e.wait_ge(sem, val)
                    else:
                        ins = rec[1](e)
                        if rec[2] is not None:
                            kind, key = rec[2]
                            if kind == "ms":
                                ins.then_inc(s.sem[key], 1)
                            else:
                                ins.then_inc(s.dsem[key], 16)
            return body
        block.tensor(replay("pe", s.ops["pe"]))
        block.scalar(replay("act", s.ops["act"]))
        block.vector(replay("dve", s.ops["dve"]))
        block.gpsimd(replay("pool", s.ops["pool"]))
        block.sync(replay("sp", s.ops["sp"]))


def build_nc(cfg):
    c = cfg
    D, KC, FC, H, NP, TT, NCH, NH, DEPTH = c.D, c.KC, c.FC, c.H, c.NP, c.TT, c.NCH, c.NH, c.DEPTH
    nc = bass.Bass("TRN2", target_bir_lowering=False)
    es = ExitStack()
    xin = nc.dram_tensor("xin", [KC, 128, TT], F32, kind="ExternalInput")
    consts_d = nc.dram_tensor("consts", [128, c.CW], F32, kind="ExternalInput")
    wgu = {}
    wd = {}
    for l in range(DEPTH):
        for f in "ab":
            wgu[l, f] = nc.dram_tensor(f"wgu_{l}{f}", [FC, 128, 2 * KC * 128], F32, kind="ExternalInput")
            wd[l, f] = nc.dram_tensor(f"wd_{l}{f}", [3 * KC, 128, c.PSMAX * 128], F32, kind="ExternalInput")
    win = [nc.dram_tensor(f"win_{l}", [NP, 128, KC * 128], F32, kind="ExternalInput") for l in range(DEPTH)]
    wout = [nc.dram_tensor(f"wout_{l}", [KC, 128, KC * 128], F32, kind="ExternalInput") for l in range(DEPTH)]
    y = nc.dram_tensor("y", [KC, 128, TT], F32, kind="ExternalOutput")
    XS = nc.dram_tensor("xs", [KC, 128, TT], F32)
    AT = nc.dram_tensor("at", [FC, 128, TT], BF16)
    PT = nc.dram_tensor("pt", [NP, 128, TT], F32, **({"kind": "ExternalOutput"} if c.debug else {}))
    DBR = nc.dram_tensor("dbr", [KC, 128, TT], BF16, kind="ExternalOutput") if c.debug else None
    KVS = nc.dram_tensor("kvs", [H, 128, TT], F32)
    INTRA = nc.dram_tensor("intra", [H, 128, TT], F32)
    QXS = nc.dram_tensor("qxs", [H, 128, TT], BF16)
    CVO = nc.dram_tensor("cvo", [H, 128, TT], BF16)
    EXI = [nc.dram_tensor(f"exi{l}", [128, c.EW], F32) for l in range(DEPTH)]
    EXO = [nc.dram_tensor(f"exo{l}", [256, c.EW], F32) for l in range(DEPTH)]

    P = Prog(nc, es)
    sb = lambda name, shape, dt: es.enter_context(nc.sbuf_tensor(name, shape, dt))
    ACTB = sb("actb", [128, KC, TT], BF16)
    NSLOT = 3
    WB = [sb(f"wb{i}", [128, c.WSLOT], BF16) for i in range(NSLOT)]
    CON = sb("con", [128, c.CW], F32)
    FT = [sb(f"ft{i}", [128, TT], F32) for i in range(7)]
    BT = [sb(f"bt{i}", [128, TT], BF16) for i in range(7)]
    SBALL = sb("sball", [128, TT], BF16)
    SIN = sb("sin", [128, c.EW], F32)
    SST = sb("sst", [128, (NCH + 1) * 128], F32)
    CU = sb("cu", [128, TT + 2], F32)
    HALO = sb("halo", [128, H, 2], F32)
    Y2 = sb("y2", [128, H, 2], F32)
    B2 = sb("b2", [128, H, 2], F32)
    FX = sb("fx", [128, H, 2], F32)
    IDB = sb("idb", [128, 128], BF16)
    PS = [es.enter_context(nc.psum_tensor(f"ps{i}", [128, TT], F32)) for i in range(4)]

    wchunks = []
    for l in range(DEPTH):
        def ffn_chunks(f):
            for fc in range(FC):
                wchunks.append((wgu[l, f][fc, :, :], 2 * KC * 128))
            for pi, ps_ in enumerate(c.parts):
                for dc in range(KC):
                    wchunks.append((wd[l, f][pi * KC + dc, :, 0:ps_ * 128], ps_ * 128))
        ffn_chunks("a")
        if c.mixer:
            for cc in ([H + h for h in range(H)] + [2 * H + h for h in range(H)] + [h for h in range(H)] +
                       [5 * H + j for j in range(H)] + [6 * H + j for j in range(H)] + [4 * H + j for j in range(H)] +
                       [3 * H + h for h in range(H)]):
                wchunks.append((win[l][cc, :, :], KC * 128))
            for dc in range(KC):
                wchunks.append((wout[l][dc, :, :], KC * 128))
        ffn_chunks("b")
    wsem = [P.dma_sem(f"w{i}") for i in range(NSLOT)]
    wstate = {"issued": 0, "consumed": 0, "ltok": {}}

    def w_issue(free_tok):
        i = wstate["issued"]
        if i >= len(wchunks):
            return
        ap, n = wchunks[i]
        slot = i % NSLOT
        tok = P.op("pool", lambda e, ap=ap, n=n, slot=slot: e.dma_start(out=WB[slot][:, 0:n], in_=ap),
                   deps=[free_tok], dsem=wsem[slot], track=False)
        wstate["ltok"][i] = tok
        wstate["issued"] = i + 1

    def w_next():
        i = wstate["consumed"]
        wstate["consumed"] = i + 1
        return i % NSLOT, wstate["ltok"].pop(i)

    for _ in range(NSLOT):
        w_issue(None)

    psfree = [None] * 4

    def mm_group(ps, slot, woff, kcn, rhs_of, deps, mid=None, kdeps=None):
        tok = None
        if mid is not None:
            for hf in range(NH):
                for kc in range(kcn):
                    last = (kc == kcn - 1) and (hf == NH - 1)
                    tok = P.op(
                        "pe",
                        lambda e, kc=kc, hf=hf: e.matmul(
                            out=ps[:, hf * 512:(hf + 1) * 512],
                            lhsT=WB[slot][:, woff + kc * 128: woff + (kc + 1) * 128],
                            rhs=rhs_of(kc)[:, hf * 512:(hf + 1) * 512],
                            start=(kc == 0), stop=(kc == kcn - 1)),
                        deps=deps if (kc == 0 and hf == 0) else (), ms=last)
                if hf < NH - 1:
                    mid()
            return tok
        for kc in range(kcn):
            for hf in range(NH):
                last = (kc == kcn - 1) and (hf == NH - 1)
                d_ = list(deps) if (kc == 0 and hf == 0) else []
                if kdeps is not None and hf == 0:
                    d_.append(kdeps(kc))
                tok = P.op(
                    "pe",
                    lambda e, kc=kc, hf=hf: e.matmul(
                        out=ps[:, hf * 512:(hf + 1) * 512],
                        lhsT=WB[slot][:, woff + kc * 128: woff + (kc + 1) * 128],
                        rhs=rhs_of(kc)[:, hf * 512:(hf + 1) * 512],
                        start=(kc == 0), stop=(kc == kcn - 1)),
                    deps=d_, ms=last)
        return tok

    con = lambda off, n=1: CON[:, off:off + n]

    ctok = P.op("sp", lambda e: e.dma_start(out=CON[:, :], in_=consts_d[:, :]), dsem=P.dma_sem("cld"))
    t_idb = P.op("dve", lambda e: e.tensor_copy(out=IDB[:, :], in_=con(c.c_ident, 128)), deps=[ctok])
    P.barrier()

    nld = [P.dma_sem(f"nld{i}") for i in range(3)]
    nst = [P.dma_sem(f"nst{i}") for i in range(2)]

    def stage_norm(xsrc, gidx, final=False, have_acc=False):
        ring = [FT[0], FT[1], FT[2]]
        SQ, ACC, RSTD = FT[3], FT[4], FT[5]
        free = [None] * 3
        ld = {}

        def load(i):
            kc = i % KC
            s_ = i % 3
            ld[i] = P.op("sp", lambda e, kc=kc, s_=s_: e.dma_start(out=ring[s_][:, :], in_=xsrc[kc, :, :]),
                         deps=[free[s_]], dsem=nld[s_])
        n2 = 2 * KC
        i0 = KC if have_acc else 0
        for i in range(i0, min(i0 + 2, n2)):
            load(i)
        acc_tok = None
        rstd_tok = None
        ofree = [None, None]
        if have_acc:
            tm = None
            for hf in range(NH):
                tm = P.op("pe", lambda e, hf=hf: e.matmul(
                    out=PS[0][:, hf * 512:(hf + 1) * 512], lhsT=con(c.c_onesd, 128),
                    rhs=ACC[:, hf * 512:(hf + 1) * 512], start=True, stop=True), ms=(hf == NH - 1))
            tsq_ = P.op("act", lambda e: e.activation(out=RSTD[:, :], in_=PS[0][:, :], func=AF.Sqrt,
                                                      bias=con(c.c_eps), scale=1.0), deps=[tm])
            rstd_tok = P.op("dve", lambda e: e.reciprocal(out=RSTD[:, :], in_=RSTD[:, :]), deps=[tsq_])
        for i in range(i0, n2):
            if i + 2 < n2:
                load(i + 2)
            kc = i % KC
            s_ = i % 3
            if i < KC:
                if kc == 0:
                    t1 = P.op("act", lambda e, s_=s_: e.activation(out=ACC[:, :], in_=ring[s_][:, :], func=AF.Square),
                              deps=[ld[i]])
                    acc_tok = t1
                    free[s_] = t1
                else:
                    t1 = P.op("act", lambda e, s_=s_: e.activation(out=SQ[:, :], in_=ring[s_][:, :], func=AF.Square),
                              deps=[ld[i], acc_tok])
                    free[s_] = t1
                    acc_tok = P.op("dve", lambda e: e.tensor_tensor(out=ACC[:, :], in0=ACC[:, :], in1=SQ[:, :], op=ALU.add),
                                   deps=[t1, acc_tok])
                if kc == KC - 1:
                    tm = None
                    for hf in range(NH):
                        tm = P.op("pe", lambda e, hf=hf: e.matmul(
                            out=PS[0][:, hf * 512:(hf + 1) * 512], lhsT=con(c.c_onesd, 128),
                            rhs=ACC[:, hf * 512:(hf + 1) * 512], start=True, stop=True),
                            deps=[acc_tok], ms=(hf == NH - 1))
                    tsq_ = P.op("act", lambda e: e.activation(out=RSTD[:, :], in_=PS[0][:, :], func=AF.Sqrt,
                                                              bias=con(c.c_eps), scale=1.0), deps=[tm])
                    rstd_tok = P.op("dve", lambda e: e.reciprocal(out=RSTD[:, :], in_=RSTD[:, :]), deps=[tsq_])
            else:
                gcol = c.c_norm + gidx * KC + kc
                if not final:
                    t2 = P.op("dve", lambda e, s_=s_, kc=kc, gcol=gcol: e.scalar_tensor_tensor(
                        out=ACTB[:, kc, :], in0=ring[s_][:, :], scalar=con(gcol), in1=RSTD[:, :],
                        op0=ALU.mult, op1=ALU.mult), deps=[ld[i], rstd_tok])
                    free[s_] = t2
                else:
                    o_ = kc % 2
                    OT = [FT[3], FT[4]][o_]
                    t2 = P.op("dve", lambda e, s_=s_, OT=OT, gcol=gcol: e.scalar_tensor_tensor(
                        out=OT[:, :], in0=ring[s_][:, :], scalar=con(gcol), in1=RSTD[:, :],
                        op0=ALU.mult, op1=ALU.mult), deps=[ld[i], rstd_tok, ofree[o_]])
                    free[s_] = t2
                    ofree[o_] = P.op("sp", lambda e, OT=OT, kc=kc: e.dma_start(out=y[kc, :, :], in_=OT[:, :]),
                                     deps=[t2], dsem=nst[o_])
        P.barrier()

    ast_sem = [P.dma_sem(f"ast{i}") for i in range(3)]

    def stage_gateup():
        SG = [FT[0], FT[1]]
        AST = [BT[0], BT[1], BT[2]]
        sgfree = [None, None]
        astfree = [None] * 3
        for fc in range(FC):
            slot, ltok = w_next()
            pg, pu = PS[(2 * fc) % 4], PS[(2 * fc + 1) % 4]
            ig, iu = (2 * fc) % 4, (2 * fc + 1) % 4
            tg = mm_group(pg, slot, 0, KC, lambda kc: ACTB[:, kc, :], [ltok, psfree[ig]])
            tu = mm_group(pu, slot, KC * 128, KC, lambda kc: ACTB[:, kc, :], [psfree[iu]])
            w_issue(tu)
            i_ = fc % 2
            j_ = fc % 3
            ta = P.op("act", lambda e, pg=pg, i_=i_: e.activation(out=SG[i_][:, :], in_=pg[:, :], func=AF.Silu),
                      deps=[tg, sgfree[i_]])
            psfree[ig] = ta
            tv = P.op("dve", lambda e, pu=pu, i_=i_, j_=j_: e.tensor_tensor(
                out=AST[j_][:, :], in0=pu[:, :], in1=SG[i_][:, :], op=ALU.mult), deps=[tu, ta, astfree[j_]])
            psfree[iu] = tv
            sgfree[i_] = tv
            astfree[j_] = P.op("sp", lambda e, fc=fc, j_=j_: e.dma_start(out=AT[fc, :, :], in_=AST[j_][:, :]),
                               deps=[tv], dsem=ast_sem[j_])
        P.barrier()
        for i in range(4):
            psfree[i] = None

    xld = [P.dma_sem(f"xld{i}") for i in range(3)]
    xst = [P.dma_sem(f"xst{i}") for i in range(3)]
    atl = [P.dma_sem(f"atl{i}") for i in range(4)]

    def stage_resid(xsrc, parts, scale, load_at):
        XT = [FT[0], FT[1], FT[2]]
        SQ, ACC = FT[3], FT[4]
        xfree = [None] * 3
        sqfree = [None] * 3
        acc_tok = [None]
        stok = {}
        fc0 = 0
        for pi, ps_ in enumerate(parts):
            atok = None
            ktok = {}
            if load_at:
                k0 = 0
                di = 0
                while k0 < ps_:
                    k1 = min(ps_, k0 + 8)
                    t_ = P.op("sp", lambda e, k0=k0, k1=k1, fc0=fc0: e.dma_start(
                        out=ACTB[:, k0:k1, :], in_=AT[fc0 + k0:fc0 + k1, :, :].rearrange("c p t -> p c t")),
                        dsem=atl[di % 4])
                    for kk in range(k0, k1):
                        ktok[kk] = t_
                    k0 = k1
                    di += 1
            src = xsrc if pi == 0 else XS
            ld = {}

            def load(dc):
                s_ = dc % 3
                ld[dc] = P.op("sp", lambda e, dc=dc, s_=s_, src=src: e.dma_start(out=XT[s_][:, :], in_=src[dc, :, :]),
                              deps=[xfree[s_], sqfree[s_], stok.get(dc)], dsem=xld[s_])
            for dc in range(min(2, KC)):
                load(dc)
            for dc in range(KC):
                if dc + 2 < KC:
                    load(dc + 2)
                slot, ltok = w_next()
                ip = dc % 4
                tm = mm_group(PS[ip], slot, 0, ps_, lambda kc: ACTB[:, kc, :], [ltok, psfree[ip], atok],
                              kdeps=((lambda kc: ktok.get(kc)) if (load_at and dc == 0) else None))
                w_issue(tm)
                s_ = dc % 3
                tv = P.op("dve", lambda e, ip=ip, s_=s_: e.scalar_tensor_tensor(
                    out=XT[s_][:, :], in0=PS[ip][:, :], scalar=float(scale), in1=XT[s_][:, :],
                    op0=ALU.mult, op1=ALU.add), deps=[tm, ld[dc]])
                psfree[ip] = tv
                stok[dc] = P.op("sp", lambda e, dc=dc, s_=s_: e.dma_start(out=XS[dc, :, :], in_=XT[s_][:, :]),
                                deps=[tv], dsem=xst[s_])
                xfree[s_] = stok[dc]
                if pi == len(parts) - 1:
                    if dc == 0:
                        t1 = P.op("act", lambda e, s_=s_: e.activation(out=ACC[:, :], in_=XT[s_][:, :], func=AF.Square),
                                  deps=[tv])
                        acc_tok[0] = t1
                    else:
                        t1 = P.op("act", lambda e, s_=s_: e.activation(out=SQ[:, :], in_=XT[s_][:, :], func=AF.Square),
                                  deps=[tv, acc_tok[0]])
                        acc_tok[0] = P.op("dve", lambda e: e.tensor_tensor(out=ACC[:, :], in0=ACC[:, :], in1=SQ[:, :],
                                                                           op=ALU.add), deps=[t1, acc_tok[0]])
                    sqfree[s_] = t1
            fc0 += ps_
            P.barrier()
            for i in range(4):
                psfree[i] = None
            for i in range(3):
                xfree[i] = None

    pst = [P.dma_sem(f"pst{i}") for i in range(2)]
    ml = [P.dma_sem(f"ml{i}") for i in range(8)]
    mst = [P.dma_sem(f"mst{i}") for i in range(4)]
    exs = P.dma_sem("exs")
    exl = P.dma_sem("exl")
    cvl = P.dma_sem("cvl")
    ccs = [es.enter_context(nc.semaphore(f"cc{l}")) for l in range(DEPTH)]
    L0, L1, T0, T1, T2, T3, T4 = FT
    KB, VB, KZ, VT, QB, QX, ST = BT
    gam = [1.0 - 2.0 ** (-5 - h) for h in range(H)]
    g128 = [float(np.float64(g) ** 128) for g in gam]

    def blk(t, n):
        return t[:, n * 128:(n + 1) * 128]

    RES = {}

    def _res(k):
        if k not in RES:
            RES[k] = [None, []]
        return RES[k]

    def _deps(reads, writes, extra=()):
        deps = list(extra)
        for k in reads:
            deps.append(_res(k)[0])
        for k in writes:
            r = _res(k)
            deps.append(r[0])
            deps += r[1]
        return deps

    def _upd(tok, reads, writes):
        if tok is None:
            return
        for k in reads:
            _res(k)[1].append(tok)
        for k in writes:
            r = _res(k)
            r[0] = tok
            r[1] = []

    def auto(eng, fn, reads=(), writes=(), dsem=None, extra=()):
        tok = P.op(eng, fn, _deps(reads, writes, extra), ms=(None if dsem is None else False), dsem=dsem)
        _upd(tok, reads, writes)
        return tok

    def auto_group(fns, reads=(), writes=(), extra=()):
        deps = _deps(reads, writes, extra)
        tok = None
        if c.pedrain:
            P.op("pe", lambda e: e.drain(), deps, ms=False)
            deps = ()
        for i, fn in enumerate(fns):
            tok = P.op("pe", fn, deps if i == 0 else (), ms=(i == len(fns) - 1))
        if c.pedrain:
            P.op("pe", lambda e: e.drain(), (), ms=False)
        _upd(tok, reads, writes)
        return tok

    def res_reset():
        RES.clear()

    order_in = ([H + h for h in range(H)] + [2 * H + h for h in range(H)] + [h for h in range(H)] +
                [5 * H + j for j in range(H)] + [6 * H + j for j in range(H)] + [4 * H + j for j in range(H)] +
                [3 * H + h for h in range(H)])

    def rope_bg(src, dst, psname, ps):
        auto_group([lambda e, hf=hf: e.matmul(out=ps[:, hf * 512:(hf + 1) * 512], lhsT=con(c.c_perm, 128),
                                              rhs=src[1][:, hf * 512:(hf + 1) * 512], start=True, stop=True)
                    for hf in range(NH)], reads=[src[0]], writes=[psname])
        yield
        auto("dve", lambda e: e.tensor_tensor(out=T0[:, :], in0=src[1][:, :], in1=con(c.c_cos, TT), op=ALU.mult),
             reads=[src[0]], writes=["T0"])
        auto("dve", lambda e: e.tensor_tensor(out=T1[:, :], in0=ps[:, :], in1=con(c.c_sin, TT), op=ALU.mult),
             reads=[psname], writes=["T1"])
        auto("dve", lambda e: e.tensor_tensor(out=dst[1][:, :], in0=T0[:, :], in1=T1[:, :], op=ALU.add),
             reads=["T0", "T1"], writes=[dst[0]])

    def bg_job(l, pt_ready):
        pk = PS[1][:, 0:TT // 2].bitcast(BF16)
        pv = PS[1][:, TT // 2:TT].bitcast(BF16)
        alldone = lambda: all(pt_ready(cc) for cc in range(NP))
        kv_loaded = set()

        def load_kv(h):
            kv_loaded.add(h)
            auto("sp", lambda e, h=h: e.dma_start(out=L0[:, :], in_=PT[H + h, :, :]), reads=[("pt", H + h)],
                 writes=["L0"], dsem=ml[0])
            auto("sp", lambda e, h=h: e.dma_start(out=L1[:, :], in_=PT[2 * H + h, :, :]), reads=[("pt", 2 * H + h)],
                 writes=["L1"], dsem=ml[1])

        for h in range(H):
            while c.gate in (1, 4) and h >= (1 if c.gate == 1 else 0) and not alldone():
                yield
            while not (pt_ready(H + h) and pt_ready(2 * H + h)):
                yield
            if h not in kv_loaded:
                load_kv(h)
            yield from rope_bg(("L0", L0), ("T2", T2), "PS0", PS[0])
            q_early = pt_ready(h)
            if q_early:
                auto("sp", lambda e, h=h: e.dma_start(out=L0[:, :], in_=PT[h, :, :]), reads=[("pt", h)], writes=["L0"],
                     dsem=ml[0])
            auto("act", lambda e: e.activation(out=KB[:, :], in_=T2[:, :], func=AF.Copy), reads=["T2"], writes=["KB"])
            auto("act", lambda e: e.activation(out=VB[:, :], in_=L1[:, :], func=AF.Copy), reads=["L1"], writes=["VB"])
            yield
            if not (c.bgskip & 8 and h >= 1):
                auto_group([lambda e, n=n: e.transpose(out=blk(pk, n), in_=blk(KB, n), identity=IDB[:, :]) for n in range(NCH)] +
                           [lambda e, n=n: e.transpose(out=blk(pv, n), in_=blk(VB, n), identity=IDB[:, :]) for n in range(NCH)],
                           reads=["KB", "VB"], writes=["PS1"])
            yield
            if not (c.bgskip & 1 and h >= 1):
                auto("dve", lambda e, h=h: e.tensor_scalar(out=KZ[:, :], in0=pk, scalar1=con(c.c_zeta + h), scalar2=None,
                                                           op0=ALU.mult), reads=["PS1"], writes=["KZ"])
            if not (c.bgskip & 2 and h >= 1):
                auto("act", lambda e: e.activation(out=VT[:, :], in_=pv, func=AF.Copy), reads=["PS1"], writes=["VT"])
            if c.kvbar and h >= 1:
                P.barrier()
            if not (c.bgskip & 4 and h >= 1):
                auto_group([lambda e, n=n: e.matmul(out=blk(PS[1], n), lhsT=blk(KZ, n), rhs=blk(VT, n), start=True, stop=True)
                            for n in range(NCH)], reads=["KZ", "VT"], writes=["PS1"])
            if not q_early:
                yield
            auto("act", lambda e: e.activation(out=T2[:, :], in_=PS[1][:, :], func=AF.Copy), reads=["PS1"], writes=["T2"])
            auto("sp", lambda e, h=h: e.dma_start(out=KVS[h, :, :], in_=T2[:, :]), reads=["T2"], writes=[("kvs", h)],
                 dsem=mst[0])
            auto("dve", lambda e, h=h: e.tensor_copy(out=blk(SIN, h), in_=blk(T2, 0)), reads=["T2"], writes=["SIN"])
            for n in range(1, NCH):
                auto("dve", lambda e, h=h, n=n: e.scalar_tensor_tensor(
                    out=blk(SIN, h), in0=blk(SIN, h), scalar=g128[h], in1=blk(T2, n),
                    op0=ALU.mult, op1=ALU.add), reads=["T2", "SIN"], writes=["SIN"])
            while c.gate == 2 and h >= 1 and not alldone():
                yield
            if not q_early:
                while not pt_ready(h):
                    yield
                auto("sp", lambda e, h=h: e.dma_start(out=L0[:, :], in_=PT[h, :, :]), reads=[("pt", h)], writes=["L0"],
                     dsem=ml[0])
            yield from rope_bg(("L0", L0), ("L1", L1), "PS0", PS[0])
            auto("act", lambda e: e.activation(out=QB[:, :], in_=L1[:, :], func=AF.Copy), reads=["L1"], writes=["QB"])
            auto("dve", lambda e, h=h: e.tensor_tensor(
                out=QX[:, :].rearrange("p (n c) -> p n c", c=128), in0=L1[:, :].rearrange("p (n c) -> p n c", c=128),
                in1=con(c.c_xi + h * 128, 128).unsqueeze(1).to_broadcast([128, NCH, 128]), op=ALU.mult),
                 reads=["L1"], writes=["QX"])
            auto("sp", lambda e, h=h: e.dma_start(out=QXS[h, :, :], in_=QX[:, :]), reads=["QX"], writes=[("qxs", h)],
                 dsem=mst[1])
            auto_group([lambda e, n=n: e.matmul(out=blk(PS[1], n), lhsT=blk(KB, n), rhs=blk(QB, n), start=True, stop=True)
                        for n in range(NCH)], reads=["KB", "QB"], writes=["PS1"])
            if h + 1 < H and pt_ready(H + h + 1) and pt_ready(2 * H + h + 1):
                load_kv(h + 1)
            yield
            auto("dve", lambda e, h=h: e.tensor_tensor(
                out=ST[:, :].rearrange("p (n c) -> p n c", c=128), in0=PS[1][:, :].rearrange("p (n c) -> p n c", c=128),
                in1=con(c.c_dm + h * 128, 128).unsqueeze(1).to_broadcast([128, NCH, 128]), op=ALU.mult),
                 reads=["PS1"], writes=["ST"])
            auto_group([lambda e, n=n: e.matmul(out=blk(PS[0], n), lhsT=blk(VT, n), rhs=blk(ST, n), start=True, stop=True)
                        for n in range(NCH)], reads=["VT", "ST"], writes=["PS0"])
            yield
            auto("act", lambda e: e.activation(out=T3[:, :], in_=PS[0][:, :], func=AF.Copy), reads=["PS0"], writes=["T3"])
            auto("sp", lambda e, h=h: e.dma_start(out=INTRA[h, :, :], in_=T3[:, :]), reads=["T3"], writes=[("intra", h)],
                 dsem=mst[2])
        while c.convlate and not all(pt_ready(cc) for cc in range(NP)):
            yield
        auto("dve", lambda e: e.memset(CU[:, 0:2], 0.0), writes=["CU"])
        for j in range(H):
            while not (pt_ready(4 * H + j) and pt_ready(5 * H + j) and pt_ready(6 * H + j)):
                yield
            auto("sp", lambda e, j=j: e.dma_start(out=L0[:, :], in_=PT[5 * H + j, :, :]), reads=[("pt", 5 * H + j)],
                 writes=["L0"], dsem=ml[0])
            auto("sp", lambda e, j=j: e.dma_start(out=L1[:, :], in_=PT[6 * H + j, :, :]), reads=[("pt", 6 * H + j)],
                 writes=["L1"], dsem=ml[1])
            auto("sp", lambda e, j=j: e.dma_start(out=T2[:, :], in_=PT[4 * H + j, :, :]), reads=[("pt", 4 * H + j)],
                 writes=["T2"], dsem=ml[2])
            auto("dve", lambda e: e.tensor_tensor(out=CU[:, 2:TT + 2], in0=L0[:, :], in1=L1[:, :], op=ALU.mult),
                 reads=["L0", "L1"], writes=["CU"])
            cw = lambda tap, j=j: con(c.c_cw + (l * 3 + tap) * H + j)
            auto("dve", lambda e, j=j: e.tensor_copy(out=HALO[:, j, :], in_=CU[:, TT:TT + 2]), reads=["CU"], writes=["HALO"])
            auto("dve", lambda e, cw=cw: e.tensor_scalar(out=T0[:, :], in0=CU[:, 2:TT + 2], scalar1=cw(2), scalar2=None,
                                                        op0=ALU.mult), reads=["CU"], writes=["T0"])
            auto("dve", lambda e, cw=cw: e.scalar_tensor_tensor(out=T0[:, :], in0=CU[:, 1:TT + 1], scalar=cw(1),
                                                               in1=T0[:, :], op0=ALU.mult, op1=ALU.add),
                 reads=["CU", "T0"], writes=["T0"])
            auto("dve", lambda e, cw=cw: e.scalar_tensor_tensor(out=T0[:, :], in0=CU[:, 0:TT], scalar=cw(0),
                                                               in1=T0[:, :], op0=ALU.mult, op1=ALU.add),
                 reads=["CU", "T0"], writes=["T0"])
            auto("dve", lambda e, j=j: e.tensor_copy(out=Y2[:, j, :], in_=T0[:, 0:2]), reads=["T0"], writes=["Y2"])
            auto("dve", lambda e, j=j: e.tensor_copy(out=B2[:, j, :], in_=T2[:, 0:2]), reads=["T2"], writes=["B2"])
            auto("dve", lambda e: e.tensor_tensor(out=KB[:, :], in0=T0[:, :], in1=T2[:, :], op=ALU.mult),
                 reads=["T0", "T2"], writes=["KB"])
            auto("sp", lambda e, j=j: e.dma_start(out=CVO[j, :, :], in_=KB[:, :]), reads=["KB"], writes=[("cvo", j)],
                 dsem=mst[3])
            yield

    def stage_mix(l):
        res_reset()
        ptok = {}
        ready = lambda cc: cc in ptok
        bg = bg_job(l, ready)
        last_tm = [None]
        st_ = {"alive": not c.nobg}

        def adv():
            st_["n"] = st_.get("n", 0) + 1
            if st_["n"] > c.bgsteps:
                st_["alive"] = False
            if st_["alive"]:
                try:
                    next(bg)
                except StopIteration:
                    st_["alive"] = False

        for i, cc in enumerate(order_in):
            slot, ltok = w_next()
            ip = 2 + (i % 2)
            psn = f"PS{ip}"
            deps = _deps([], [psn], [ltok])
            tm = mm_group(PS[ip], slot, 0, KC, lambda kc: ACTB[:, kc, :], deps, mid=(adv if (i >= 2 * H and c.bg2) else None))
            _upd(tm, [], [psn])
            last_tm[0] = tm
            w_issue(tm)
            if i % 2 == 0:
                auto("act", lambda e, ip=ip: e.activation(out=T4[:, :], in_=PS[ip][:, :], func=AF.Copy),
                     reads=[psn], writes=["T4"])
            else:
                auto("dve", lambda e, ip=ip: e.tensor_copy(out=T4[:, :], in_=PS[ip][:, :]), reads=[psn], writes=["T4"])
            ptok[cc] = auto("sp", lambda e, cc=cc: e.dma_start(out=PT[cc, :, :], in_=T4[:, :]), reads=["T4"],
                            writes=[("pt", cc)], dsem=pst[i % 2])
            if i >= 2 * H - 1:
                for _ in range(c.bgn):
                    adv()
        while st_["alive"]:
            adv()
        if c.stopat == 1:
            raise StopBuild()
        auto("sp", lambda e: e.dma_start(out=ACTB[:, H:2 * H, :], in_=CVO[:, :, :].rearrange("c p t -> p c t")),
             reads=[("cvo", j) for j in range(H)], writes=["ACTBc"], dsem=cvl, extra=[last_tm[0]])
        auto("dve", lambda e: e.tensor_copy(out=SIN[:, H * 128:c.EW].rearrange("p (c t) -> p c t", t=2), in_=HALO[:, :, :]),
             reads=["HALO", "SIN"], writes=["SIN"])
        ts = auto("sp", lambda e: e.dma_start(out=EXI[l][:, :], in_=SIN[:, :]), reads=["SIN"], writes=["EXI"], dsem=exs)
        P.op("pool", lambda e: e.collective_compute(
            "AllGather", ALU.bypass, replica_groups=[[2 * i, 2 * i + 1] for i in range(NCORES // 2)],
            ins=[EXI[l].ap().opt()], outs=[EXO[l].ap().opt()]).then_inc(ccs[l]), deps=[ts], ms=False)
        P.op("pool", lambda e: e.wait_ge(ccs[l], 1), ms=False)
        tl = P.op("pool", lambda e: e.dma_start(out=SIN[:, :], in_=EXO[l][0:128, :]), dsem=exl)
        P.barrier()
        res_reset()
        auto("dve", lambda e: e.tensor_scalar(out=SIN[:, :], in0=SIN[:, :], scalar1=con(c.c_sel), scalar2=None,
                                              op0=ALU.mult), writes=["SIN"], extra=[tl])
        if c.stopat == 2:
            raise StopBuild()
        HL = SIN[:, H * 128:c.EW].rearrange("p (c t) -> p c t", t=2)
        W0 = con(c.c_cw + (l * 3 + 0) * H, H)
        W1 = con(c.c_cw + (l * 3 + 1) * H, H)
        auto("dve", lambda e: e.tensor_tensor(out=FX[:, :, 0], in0=HL[:, :, 1], in1=W1, op=ALU.mult), reads=["SIN"], writes=["FX"])
        auto("dve", lambda e: e.tensor_tensor(out=FX[:, :, 1], in0=HL[:, :, 0], in1=W0, op=ALU.mult), reads=["SIN"], writes=["FX"])
        auto("dve", lambda e: e.tensor_tensor(out=FX[:, :, 0], in0=FX[:, :, 0], in1=FX[:, :, 1], op=ALU.add), reads=["FX"], writes=["FX"])
        auto("dve", lambda e: e.tensor_tensor(out=FX[:, :, 1], in0=HL[:, :, 1], in1=W0, op=ALU.mult), reads=["SIN"], writes=["FX"])
        auto("dve", lambda e: e.tensor_tensor(out=Y2[:, :, :], in0=Y2[:, :, :], in1=FX[:, :, :], op=ALU.add), reads=["FX"], writes=["Y2"])
        auto("dve", lambda e: e.tensor_tensor(out=ACTB[:, H:2 * H, 0:2], in0=Y2[:, :, :], in1=B2[:, :, :], op=ALU.mult),
             reads=["Y2", "ACTBc"], writes=["ACTBc"])
        if c.stopat == 3:
            raise StopBuild()
        sets = [dict(I=("L0", L0), K=("L1", L1), G=("T0", T0), Q=("KB", KB), ps=("PS0", PS[0]), pn=("PS2", PS[2])),
                dict(I=("T1", T1), K=("T2", T2), G=("T3", T3), Q=("VB", VB), ps=("PS1", PS[1]), pn=("PS3", PS[3]))]

        def loads_kq(h):
            s_ = sets[h % 2]
            o = 4 * (h % 2)
            auto("sp", lambda e: e.dma_start(out=s_["K"][1][:, :], in_=KVS[h, :, :]), writes=[s_["K"][0]], dsem=ml[o + 1])
            auto("sp", lambda e: e.dma_start(out=s_["Q"][1][:, :], in_=QXS[h, :, :]), writes=[s_["Q"][0]], dsem=ml[o + 2])

        def loads_ig(h):
            s_ = sets[h % 2]
            o = 4 * (h % 2)
            auto("sp", lambda e: e.dma_start(out=s_["I"][1][:, :], in_=INTRA[h, :, :]), writes=[s_["I"][0]], dsem=ml[o + 0])
            auto("sp", lambda e: e.dma_start(out=s_["G"][1][:, :], in_=PT[3 * H + h, :, :]), writes=[s_["G"][0]], dsem=ml[o + 3])

        def part1(h):
            s_ = sets[h % 2]
            Kn, Kt = s_["K"]
            auto("dve", lambda e: e.tensor_copy(out=blk(SST, 0), in_=blk(SIN, h)), reads=["SIN"], writes=["SST"])
            for n in range(NCH):
                auto("dve", lambda e, n=n: e.scalar_tensor_tensor(
                    out=blk(SST, n + 1), in0=blk(SST, n), scalar=g128[h], in1=blk(Kt, n),
                    op0=ALU.mult, op1=ALU.add), reads=["SST", Kn], writes=["SST"])
            auto("act", lambda e: e.activation(out=SBALL[:, :], in_=SST[:, 0:NCH * 128], func=AF.Copy),
                 reads=["SST"], writes=["SBALL"])
            psn, ps = s_["ps"]
            Qn, Qt = s_["Q"]
            auto_group([lambda e, n=n: e.matmul(out=blk(ps, n), lhsT=blk(SBALL, n), rhs=blk(Qt, n), start=True, stop=True)
                        for n in range(NCH)], reads=["SBALL", Qn], writes=[psn])

        def part2(h):
            s_ = sets[h % 2]
            In, It = s_["I"]
            Gn, Gt = s_["G"]
            psn, ps = s_["ps"]
            pnn, pn = s_["pn"]
            auto("dve", lambda e: e.tensor_tensor(out=It[:, :], in0=ps[:, :], in1=It[:, :], op=ALU.add),
                 reads=[psn, In], writes=[In])
            auto("act", lambda e: e.activation(out=T4[:, :], in_=It[:, :], func=AF.Square), reads=[In], writes=["T4"])
            auto_group([lambda e, hf=hf: e.matmul(out=pn[:, hf * 512:(hf + 1) * 512], lhsT=con(c.c_onesh, 128),
                                                  rhs=T4[:, hf * 512:(hf + 1) * 512], start=True, stop=True)
                        for hf in range(NH)], reads=["T4"], writes=[pnn])
            auto("act", lambda e: e.activation(out=T4[:, :], in_=pn[:, :], func=AF.Sqrt, bias=con(c.c_eps), scale=1.0),
                 reads=[pnn], writes=["T4"])
            auto("act", lambda e: e.activation(out=Gt[:, :], in_=Gt[:, :], func=AF.Silu), reads=[Gn], writes=[Gn])
            auto("dve", lambda e: e.reciprocal(out=T4[:, :], in_=T4[:, :]), reads=["T4"], writes=["T4"])
            auto("dve", lambda e: e.tensor_tensor(out=It[:, :], in0=It[:, :], in1=T4[:, :], op=ALU.mult),
                 reads=[In, "T4"], writes=[In])
            auto("dve", lambda e: e.scalar_tensor_tensor(
                out=ACTB[:, h, :], in0=It[:, :], scalar=con(c.c_rn + l * H + h), in1=Gt[:, :],
                op0=ALU.mult, op1=ALU.mult), reads=[In, Gn], writes=[("actb", h)])

        loads_kq(0)
        loads_ig(0)
        for h in range(H + 1):
            if h + 1 < H:
                loads_kq(h + 1)
            if h < H:
                part1(h)
            if h >= 1:
                part2(h - 1)
            if h + 1 < H:
                loads_ig(h + 1)
            if c.pbser:
                P.barrier()
        P.barrier()
        res_reset()

    dbs = P.dma_sem("dbs")

    def dump_r():
        P.op("sp", lambda e: e.dma_start(out=DBR[:, :, :].rearrange("c p t -> p c t"), in_=ACTB[:, :, :]), dsem=dbs)
        P.barrier()

    xcur = xin
    try:
      for l in range(DEPTH):
          stage_norm(xcur, 3 * l + 0, have_acc=(l > 0))
          stage_gateup()
          stage_resid(xcur, c.parts, 0.5, True)
          xcur = XS
          if c.mixer:
              stage_norm(xcur, 3 * l + 1, have_acc=True)
              stage_mix(l)
              if c.debug and l == 0:
                  dump_r()
              stage_resid(xcur, [KC], 1.0, False)
          stage_norm(xcur, 3 * l + 2, have_acc=c.mixer)
          stage_gateup()
          stage_resid(xcur, c.parts, 0.5, True)
      stage_norm(xcur, 3 * DEPTH, final=True, have_acc=True)
      assert wstate["consumed"] == len(wchunks), (wstate, len(wchunks))
    except StopBuild:
      P.barrier()

    assert P.simulate(), "deadlock in program order"
    with nc.Block() as block:
        P.emit(block)
    es.close()
    return nc


def _consts(cfg, half, norms, ret_norm, conv_w):
    c = cfg
    H, TT = c.H, c.TT
    A = np.zeros((128, c.CW), np.float32)
    A[:, c.c_ident:c.c_ident + 128] = np.eye(128, dtype=np.float32)
    perm = np.zeros((128, 128), np.float32)
    for dp in range(128):
        perm[(dp + 64) % 128, dp] = 1.0
    A[:, c.c_perm:c.c_perm + 128] = perm
    A[:, c.c_onesd:c.c_onesd + 128] = 1.0 / c.D
    A[:, c.c_onesh:c.c_onesh + 128] = 1.0 / 128.0
    half_d = 64
    inv_freq = (10000.0 ** (-np.arange(half_d, dtype=np.float32) / np.float32(half_d))).astype(np.float32)
    pos = (half * TT + np.arange(TT)).astype(np.float32)
    ang = (pos[None, :] * inv_freq[:, None]).astype(np.float32)
    cos = np.cos(ang).astype(np.float32)
    sin = np.sin(ang).astype(np.float32)
    A[:, c.c_cos:c.c_cos + TT] = np.concatenate([cos, cos], 0)
    A[:, c.c_sin:c.c_sin + TT] = np.concatenate([-sin, sin], 0)
    hh = np.arange(H, dtype=np.float64)
    lg = np.log1p(-np.power(2.0, -5.0 - hh))
    idx = np.arange(128, dtype=np.float64)
    scale = 128.0 ** -0.5
    diff = idx[None, :] - idx[:, None]
    dm = np.where(diff[None] >= 0, np.exp(lg[:, None, None] * np.maximum(diff, 0.0)[None]), 0.0) * scale
    A[:, c.c_dm:c.c_dm + H * 128] = dm.transpose(1, 0, 2).reshape(128, H * 128)
    xi = np.exp(lg[:, None] * (idx + 1.0)[None, :])
    A[:, c.c_xi:c.c_xi + H * 128] = np.broadcast_to(xi.reshape(1, H * 128), (128, H * 128))
    zeta = np.exp(lg[:, None] * (127.0 - idx)[None, :]) * scale
    A[:, c.c_zeta:c.c_zeta + H] = zeta.T
    A[:, c.c_sel] = float(half)
    A[:, c.c_eps] = EPS
    for i, g in enumerate(norms):
        A[:, c.c_norm + i * c.KC:c.c_norm + (i + 1) * c.KC] = g.reshape(c.KC, 128).T
    for l in range(c.DEPTH):
        A[:, c.c_rn + l * H:c.c_rn + (l + 1) * H] = ret_norm[l].reshape(H, 128).T
        for tap in range(3):
            o = c.c_cw + (l * 3 + tap) * H
            A[:, o:o + H] = conv_w[l, tap].reshape(H, 128).T
    return A


def _lay_cols(w, KC):
    K, N = w.shape
    return np.ascontiguousarray(w.reshape(KC, 128, N // 128, 128).transpose(2, 1, 0, 3)).reshape(N // 128, 128, KC * 128)


def prepare_inputs(cfg, x, norm_ffa, w_ffa_gate, w_ffa_up, w_ffa_down, norm_mix, w_in, conv_w, ret_norm, w_out,
                   norm_ffb, w_ffb_gate, w_ffb_up, w_ffb_down, norm_final):
    c = cfg
    KC, FC, TT = c.KC, c.FC, c.TT
    shared = {}
    for l in range(c.DEPTH):
        for f, (wg, wu, wdn) in (("a", (w_ffa_gate, w_ffa_up, w_ffa_down)), ("b", (w_ffb_gate, w_ffb_up, w_ffb_down))):
            g = _lay_cols(np.asarray(wg[l]), KC).reshape(FC, 128, 1, KC * 128)
            u = _lay_cols(np.asarray(wu[l]), KC).reshape(FC, 128, 1, KC * 128)
            shared[f"wgu_{l}{f}"] = np.concatenate([g, u], axis=2).reshape(FC, 128, 2 * KC * 128)
            wdl = np.zeros((3 * KC, 128, c.PSMAX * 128), np.float32)
            r0 = 0
            wdn_l = np.asarray(wdn[l])
            for pi, ps_ in enumerate(c.parts):
                blk = wdn_l[r0 * 128:(r0 + ps_) * 128]
                wdl[pi * KC:(pi + 1) * KC, :, 0:ps_ * 128] = _lay_cols(blk, ps_)
                r0 += ps_
            shared[f"wd_{l}{f}"] = wdl
        shared[f"win_{l}"] = _lay_cols(np.asarray(w_in[l]), KC)
        shared[f"wout_{l}"] = _lay_cols(np.asarray(w_out[l]), KC)
    norms = []
    for l in range(c.DEPTH):
        norms += [np.asarray(norm_ffa[l]), np.asarray(norm_mix[l]), np.asarray(norm_ffb[l])]
    norms.append(np.asarray(norm_final))
    x = np.asarray(x)
    in_maps = []
    for core in range(NCORES):
        b, half = core // 2, core % 2
        xt = np.ascontiguousarray(x[b, half * TT:(half + 1) * TT, :].T).reshape(KC, 128, TT)
        m = dict(shared)
        m["xin"] = xt
        m["consts"] = _consts(c, half, norms, np.asarray(ret_norm), np.asarray(conv_w))
        in_maps.append(m)
    return in_maps


def run(cfg, inputs, trace=False):
    nc = build_nc(cfg)
    in_maps = prepare_inputs(cfg, **inputs)
    res = run_bass_kernel_spmd(nc, in_maps, core_ids=list(range(NCORES)), **({"trace": True} if trace else {}))
    B = NCORES // 2
    out = np.zeros((B, 2 * cfg.TT, cfg.D), np.float32)
    for core in range(NCORES):
        b, half = core // 2, core % 2
        yv = np.asarray(res.results[core]["y"]).reshape(cfg.D, cfg.TT)
        out[b, half * cfg.TT:(half + 1) * cfg.TT, :] = yv.T
    return out, res


def kernel(**inputs):
    cfg = Cfg()
    out, _ = run(cfg, inputs)
    return out
```

```python
import numpy as np
from contextlib import ExitStack
import concourse.bass as bass
import concourse.mybir as mybir
from concourse.bass_utils import run_bass_kernel_spmd

F32 = mybir.dt.float32
BF16 = mybir.dt.bfloat16
ALU = mybir.AluOpType
AF = mybir.ActivationFunctionType
EPS = 1e-6
NCORES = 8


class StopBuild(Exception):
    pass


class Cfg:
    def __init__(s, D=4096, FF=11008, DEPTH=2, TT=1024, mixer=True, debug=False):
        s.debug = debug
        import os
        s.bg2 = bool(int(os.environ.get('BG2', '1')))
        s.bgn = int(os.environ.get('BGN', '1'))
        s.gate = int(os.environ.get('BGGATE', '0'))
        s.pbser = int(os.environ.get('PBSER', '0'))
        s.stopat = int(os.environ.get('STOPAT', '0'))
        s.nobg = int(os.environ.get('NOBG', '0'))
        s.pedrain = int(os.environ.get('PEDRAIN', '0'))
        s.bgsteps = int(os.environ.get('BGSTEPS', '100000'))
        s.bgskip = int(os.environ.get('BGSKIP', '0'))
        s.kvbar = int(os.environ.get('KVBAR', '0'))
        s.kvps = int(os.environ.get('KVPS', '0'))
        s.convlate = bool(int(os.environ.get('CONVLATE', '0')))
        s.D, s.FF, s.DEPTH, s.TT = D, FF, DEPTH, TT
        s.KC = D // 128
        s.FC = FF // 128
        s.H = D // 256
        s.NP = 7 * s.H
        s.NCH = TT // 128
        s.NH = TT // 512
        npart = 3
        base, rem = divmod(s.FC, npart)
        s.parts = [base + (1 if i < rem else 0) for i in range(npart)]
        s.PSMAX = max(s.parts)
        s.mixer = mixer
        s.EW = s.H * 128 + 2 * s.H
        o = 0
        def take(n):
            nonlocal o
            r = o
            o += n
            return r
        s.c_ident = take(128)
        s.c_perm = take(128)
        s.c_onesd = take(128)
        s.c_onesh = take(128)
        s.c_cos = take(TT)
        s.c_sin = take(TT)
        s.c_dm = take(s.H * 128)
        s.c_xi = take(s.H * 128)
        s.c_zeta = take(s.H)
        s.c_sel = take(1)
        s.c_eps = take(1)
        s.c_norm = take((3 * DEPTH + 1) * s.KC)
        s.c_rn = take(DEPTH * s.H)
        s.c_cw = take(DEPTH * 3 * s.H)
        s.CW = o
        s.WSLOT = max(2 * s.KC * 128, s.PSMAX * 128)


class Prog:
    ENG = ("pe", "act", "dve", "pool", "sp")

    def __init__(s, nc, es):
        s.nc, s.es = nc, es
        s.ops = {e: [] for e in s.ENG}
        s.sem = {e: es.enter_context(nc.semaphore("ms_" + e)) for e in ("pe", "act", "dve")}
        s.cnt = {e: 0 for e in s.ENG}
        s.waited = {e: {} for e in s.ENG}
        s.dsem, s.dcnt = {}, {}
        s.lastop = {e: None for e in s.ENG}
        s.pending_dma = []

    def dma_sem(s, name):
        s.dsem[name] = s.es.enter_context(s.nc.semaphore(name))
        s.dcnt[name] = 0
        return name

    def _wait(s, eng, tok):
        if tok is None:
            return
        kind, key, val = tok
        if eng == "pe" and kind == "ms" and key == "pe":
            return
        w = s.waited[eng]
        if w.get((kind, key), 0) >= val:
            return
        w[(kind, key)] = val
        s.ops[eng].append(["wait", kind, key, val])

    def _wait_all(s, eng, deps):
        best = {}
        for d in deps:
            for t in (d if isinstance(d, list) else [d]):
                if t is None:
                    continue
                k = (t[0], t[1])
                if k not in best or best[k][2] < t[2]:
                    best[k] = t
        for t in best.values():
            s._wait(eng, t)

    def op(s, eng, fn, deps=(), ms=None, dsem=None, track=True):
        if ms is None:
            ms = eng in ("act", "dve")
        s._wait_all(eng, deps)
        rec = ["op", fn, None]
        tok = None
        if dsem is not None:
            s.dcnt[dsem] += 16
            tok = ("dma", dsem, s.dcnt[dsem])
            rec[2] = ("dma", dsem)
            if track:
                s.pending_dma.append(tok)
        elif ms:
            s.cnt[eng] += 1
            tok = ("ms", eng, s.cnt[eng])
            rec[2] = ("ms", eng)
        s.ops[eng].append(rec)
        s.lastop[eng] = (rec, tok)
        return tok

    def last_tok(s, eng):
        lo = s.lastop[eng]
        if lo is None:
            return None
        rec, tok = lo
        if tok is None and rec[2] is None:
            s.cnt[eng] += 1
            tok = ("ms", eng, s.cnt[eng])
            rec[2] = ("ms", eng)
            s.lastop[eng] = (rec, tok)
        return tok

    def barrier(s):
        toks = [s.last_tok(e) for e in ("pe", "act", "dve")] + s.pending_dma
        s.pending_dma = []
        for e in ("pe", "act", "dve", "sp"):
            s._wait_all(e, [t for t in toks if t is not None and not (t[0] == "ms" and t[1] == e and e == "pe")])

    def simulate(s):
        val = {}
        pos = {e: 0 for e in s.ENG}
        while True:
            prog = False
            for e in s.ENG:
                lst = s.ops[e]
                while pos[e] < len(lst):
                    rec = lst[pos[e]]
                    if rec[0] == "wait":
                        if val.get((rec[1], rec[2]), 0) >= rec[3]:
                            pos[e] += 1
                            prog = True
                        else:
                            break
                    else:
                        if rec[2] is not None:
                            val[rec[2]] = val.get(rec[2], 0) + (1 if rec[2][0] == "ms" else 16)
                        pos[e] += 1
                        prog = True
            if all(pos[e] == len(s.ops[e]) for e in s.ENG):
                return True
            if not prog:
                for e in s.ENG:
                    if pos[e] < len(s.ops[e]):
                        print("DEADLOCK", e, "at", pos[e], "/", len(s.ops[e]), s.ops[e][pos[e]][:4],
                              "cur", val.get((s.ops[e][pos[e]][1], s.ops[e][pos[e]][2])) if s.ops[e][pos[e]][0] == "wait" else "")
                return False

    def emit(s, block):
        def replay(eng, lst):
            def body(e):
                for rec in lst:
                    if rec[0] == "wait":
                        _, kind, key, val = rec
                        sem = s.sem[key] if kind == "ms" else s.dsem[key]
                        e.wait_ge(sem, val)
                    else:
                        ins = rec[1](e)
                        if rec[2] is not None:
                            kind, key = rec[2]
                            if kind == "ms":
                                ins.then_inc(s.sem[key], 1)
                            else:
                                ins.then_inc(s.dsem[key], 16)
            return body
        block.tensor(replay("pe", s.ops["pe"]))
        block.scalar(replay("act", s.ops["act"]))
        block.vector(replay("dve", s.ops["dve"]))
        block.gpsimd(replay("pool", s.ops["pool"]))
        block.sync(replay("sp", s.ops["sp"]))


def build_nc(cfg):
    c = cfg
    D, KC, FC, H, NP, TT, NCH, NH, DEPTH = c.D, c.KC, c.FC, c.H, c.NP, c.TT, c.NCH, c.NH, c.DEPTH
    nc = bass.Bass("TRN2", target_bir_lowering=False)
    es = ExitStack()
    xin = nc.dram_tensor("xin", [KC, 128, TT], F32, kind="ExternalInput")
    consts_d = nc.dram_tensor("consts", [128, c.CW], F32, kind="ExternalInput")
    wgu = {}
    wd = {}
    for l in range(DEPTH):
        for f in "ab":
            wgu[l, f] = nc.dram_tensor(f"wgu_{l}{f}", [FC, 128, 2 * KC * 128], F32, kind="ExternalInput")
            wd[l, f] = nc.dram_tensor(f"wd_{l}{f}", [3 * KC, 128, c.PSMAX * 128], F32, kind="ExternalInput")
    win = [nc.dram_tensor(f"win_{l}", [NP, 128, KC * 128], F32, kind="ExternalInput") for l in range(DEPTH)]
    wout = [nc.dram_tensor(f"wout_{l}", [KC, 128, KC * 128], F32, kind="ExternalInput") for l in range(DEPTH)]
    y = nc.dram_tensor("y", [KC, 128, TT], F32, kind="ExternalOutput")
    XS = nc.dram_tensor("xs", [KC, 128, TT], F32)
    AT = nc.dram_tensor("at", [FC, 128, TT], BF16)
    PT = nc.dram_tensor("pt", [NP, 128, TT], F32, **({"kind": "ExternalOutput"} if c.debug else {}))
    DBR = nc.dram_tensor("dbr", [KC, 128, TT], BF16, kind="ExternalOutput") if c.debug else None
    KVS = nc.dram_tensor("kvs", [H, 128, TT], F32)
    INTRA = nc.dram_tensor("intra", [H, 128, TT], F32)
    QXS = nc.dram_tensor("qxs", [H, 128, TT], BF16)
    CVO = nc.dram_tensor("cvo", [H, 128, TT], BF16)
    EXI = [nc.dram_tensor(f"exi{l}", [128, c.EW], F32) for l in range(DEPTH)]
    EXO = [nc.dram_tensor(f"exo{l}", [256, c.EW], F32) for l in range(DEPTH)]

    P = Prog(nc, es)
    sb = lambda name, shape, dt: es.enter_context(nc.sbuf_tensor(name, shape, dt))
    ACTB = sb("actb", [128, KC, TT], BF16)
    NSLOT = 3
    WB = [sb(f"wb{i}", [128, c.WSLOT], BF16) for i in range(NSLOT)]
    CON = sb("con", [128, c.CW], F32)
    FT = [sb(f"ft{i}", [128, TT], F32) for i in range(7)]
    BT = [sb(f"bt{i}", [128, TT], BF16) for i in range(7)]
    SBALL = sb("sball", [128, TT], BF16)
    SIN = sb("sin", [128, c.EW], F32)
    SST = sb("sst", [128, (NCH + 1) * 128], F32)
    CU = sb("cu", [128, TT + 2], F32)
    HALO = sb("halo", [128, H, 2], F32)
    Y2 = sb("y2", [128, H, 2], F32)
    B2 = sb("b2", [128, H, 2], F32)
    FX = sb("fx", [128, H, 2], F32)
    IDB = sb("idb", [128, 128], BF16)
    PS = [es.enter_context(nc.psum_tensor(f"ps{i}", [128, TT], F32)) for i in range(4)]

    wchunks = []
    for l in range(DEPTH):
        def ffn_chunks(f):
            for fc in range(FC):
                wchunks.append((wgu[l, f][fc, :, :], 2 * KC * 128))
            for pi, ps_ in enumerate(c.parts):
                for dc in range(KC):
                    wchunks.append((wd[l, f][pi * KC + dc, :, 0:ps_ * 128], ps_ * 128))
        ffn_chunks("a")
        if c.mixer:
            for cc in ([H + h for h in range(H)] + [2 * H + h for h in range(H)] + [h for h in range(H)] +
                       [5 * H + j for j in range(H)] + [6 * H + j for j in range(H)] + [4 * H + j for j in range(H)] +
                       [3 * H + h for h in range(H)]):
                wchunks.append((win[l][cc, :, :], KC * 128))
            for dc in range(KC):
                wchunks.append((wout[l][dc, :, :], KC * 128))
        ffn_chunks("b")
    wsem = [P.dma_sem(f"w{i}") for i in range(NSLOT)]
    wstate = {"issued": 0, "consumed": 0, "ltok": {}}

    def w_issue(free_tok):
        i = wstate["issued"]
        if i >= len(wchunks):
            return
        ap, n = wchunks[i]
        slot = i % NSLOT
        tok = P.op("pool", lambda e, ap=ap, n=n, slot=slot: e.dma_start(out=WB[slot][:, 0:n], in_=ap),
                   deps=[free_tok], dsem=wsem[slot], track=False)
        wstate["ltok"][i] = tok
        wstate["issued"] = i + 1

    def w_next():
        i = wstate["consumed"]
        wstate["consumed"] = i + 1
        return i % NSLOT, wstate["ltok"].pop(i)

    for _ in range(NSLOT):
        w_issue(None)

    psfree = [None] * 4

    def mm_group(ps, slot, woff, kcn, rhs_of, deps, mid=None, kdeps=None):
        tok = None
        if mid is not None:
            for hf in range(NH):
                for kc in range(kcn):
                    last = (kc == kcn - 1) and (hf == NH - 1)
                    tok = P.op(
                        "pe",
                        lambda e, kc=kc, hf=hf: e.matmul(
                            out=ps[:, hf * 512:(hf + 1) * 512],
                            lhsT=WB[slot][:, woff + kc * 128: woff + (kc + 1) * 128],
                            rhs=rhs_of(kc)[:, hf * 512:(hf + 1) * 512],
                            start=(kc == 0), stop=(kc == kcn - 1)),
                        deps=deps if (kc == 0 and hf == 0) else (), ms=last)
                if hf < NH - 1:
                    mid()
            return tok
        for kc in range(kcn):
            for hf in range(NH):
                last = (kc == kcn - 1) and (hf == NH - 1)
                d_ = list(deps) if (kc == 0 and hf == 0) else []
                if kdeps is not None and hf == 0:
                    d_.append(kdeps(kc))
                tok = P.op(
                    "pe",
                    lambda e, kc=kc, hf=hf: e.matmul(
                        out=ps[:, hf * 512:(hf + 1) * 512],
                        lhsT=WB[slot][:, woff + kc * 128: woff + (kc + 1) * 128],
                        rhs=rhs_of(kc)[:, hf * 512:(hf + 1) * 512],
                        start=(kc == 0), stop=(kc == kcn - 1)),
                    deps=d_, ms=last)
        return tok

    con = lambda off, n=1: CON[:, off:off + n]

    ctok = P.op("sp", lambda e: e.dma_start(out=CON[:, :], in_=consts_d[:, :]), dsem=P.dma_sem("cld"))
    t_idb = P.op("dve", lambda e: e.tensor_copy(out=IDB[:, :], in_=con(c.c_ident, 128)), deps=[ctok])
    P.barrier()

    nld = [P.dma_sem(f"nld{i}") for i in range(3)]
    nst = [P.dma_sem(f"nst{i}") for i in range(2)]

    def stage_norm(xsrc, gidx, final=False, have_acc=False):
        ring = [FT[0], FT[1], FT[2]]
        SQ, ACC, RSTD = FT[3], FT[4], FT[5]
        free = [None] * 3
        ld = {}

        def load(i):
            kc = i % KC
            s_ = i % 3
            ld[i] = P.op("sp", lambda e, kc=kc, s_=s_: e.dma_start(out=ring[s_][:, :], in_=xsrc[kc, :, :]),
                         deps=[free[s_]], dsem=nld[s_])
        n2 = 2 * KC
        i0 = KC if have_acc else 0
        for i in range(i0, min(i0 + 2, n2)):
            load(i)
        acc_tok = None
        rstd_tok = None
        ofree = [None, None]
        if have_acc:
            tm = None
            for hf in range(NH):
                tm = P.op("pe", lambda e, hf=hf: e.matmul(
                    out=PS[0][:, hf * 512:(hf + 1) * 512], lhsT=con(c.c_onesd, 128),
                    rhs=ACC[:, hf * 512:(hf + 1) * 512], start=True, stop=True), ms=(hf == NH - 1))
            tsq_ = P.op("act", lambda e: e.activation(out=RSTD[:, :], in_=PS[0][:, :], func=AF.Sqrt,
                                                      bias=con(c.c_eps), scale=1.0), deps=[tm])
            rstd_tok = P.op("dve", lambda e: e.reciprocal(out=RSTD[:, :], in_=RSTD[:, :]), deps=[tsq_])
        for i in range(i0, n2):
            if i + 2 < n2:
                load(i + 2)
            kc = i % KC
            s_ = i % 3
            if i < KC:
                if kc == 0:
                    t1 = P.op("act", lambda e, s_=s_: e.activation(out=ACC[:, :], in_=ring[s_][:, :], func=AF.Square),
                              deps=[ld[i]])
                    acc_tok = t1
                    free[s_] = t1
                else:
                    t1 = P.op("act", lambda e, s_=s_: e.activation(out=SQ[:, :], in_=ring[s_][:, :], func=AF.Square),
                              deps=[ld[i], acc_tok])
                    free[s_] = t1
                    acc_tok = P.op("dve", lambda e: e.tensor_tensor(out=ACC[:, :], in0=ACC[:, :], in1=SQ[:, :], op=ALU.add),
                                   deps=[t1, acc_tok])
                if kc == KC - 1:
                    tm = None
                    for hf in range(NH):
                        tm = P.op("pe", lambda e, hf=hf: e.matmul(
                            out=PS[0][:, hf * 512:(hf + 1) * 512], lhsT=con(c.c_onesd, 128),
                            rhs=ACC[:, hf * 512:(hf + 1) * 512], start=True, stop=True),
                            deps=[acc_tok], ms=(hf == NH - 1))
                    tsq_ = P.op("act", lambda e: e.activation(out=RSTD[:, :], in_=PS[0][:, :], func=AF.Sqrt,
                                                              bias=con(c.c_eps), scale=1.0), deps=[tm])
                    rstd_tok = P.op("dve", lambda e: e.reciprocal(out=RSTD[:, :], in_=RSTD[:, :]), deps=[tsq_])
            else:
                gcol = c.c_norm + gidx * KC + kc
                if not final:
                    t2 = P.op("dve", lambda e, s_=s_, kc=kc, gcol=gcol: e.scalar_tensor_tensor(
                        out=ACTB[:, kc, :], in0=ring[s_][:, :], scalar=con(gcol), in1=RSTD[:, :],
                        op0=ALU.mult, op1=ALU.mult), deps=[ld[i], rstd_tok])
                    free[s_] = t2
                else:
                    o_ = kc % 2
                    OT = [FT[3], FT[4]][o_]
                    t2 = P.op("dve", lambda e, s_=s_, OT=OT, gcol=gcol: e.scalar_tensor_tensor(
                        out=OT[:, :], in0=ring[s_][:, :], scalar=con(gcol), in1=RSTD[:, :],
                        op0=ALU.mult, op1=ALU.mult), deps=[ld[i], rstd_tok, ofree[o_]])
                    free[s_] = t2
                    ofree[o_] = P.op("sp", lambda e, OT=OT, kc=kc: e.dma_start(out=y[kc, :, :], in_=OT[:, :]),
                                     deps=[t2], dsem=nst[o_])
        P.barrier()

    ast_sem = [P.dma_sem(f"ast{i}") for i in range(3)]

    def stage_gateup():
        SG = [FT[0], FT[1]]
        AST = [BT[0], BT[1], BT[2]]
        sgfree = [None, None]
        astfree = [None] * 3
        for fc in range(FC):
            slot, ltok = w_next()
            pg, pu = PS[(2 * fc) % 4], PS[(2 * fc + 1) % 4]
            ig, iu = (2 * fc) % 4, (2 * fc + 1) % 4
            tg = mm_group(pg, slot, 0, KC, lambda kc: ACTB[:, kc, :], [ltok, psfree[ig]])
            tu = mm_group(pu, slot, KC * 128, KC, lambda kc: ACTB[:, kc, :], [psfree[iu]])
            w_issue(tu)
            i_ = fc % 2
            j_ = fc % 3
            ta = P.op("act", lambda e, pg=pg, i_=i_: e.activation(out=SG[i_][:, :], in_=pg[:, :], func=AF.Silu),
                      deps=[tg, sgfree[i_]])
            psfree[ig] = ta
            tv = P.op("dve", lambda e, pu=pu, i_=i_, j_=j_: e.tensor_tensor(
                out=AST[j_][:, :], in0=pu[:, :], in1=SG[i_][:, :], op=ALU.mult), deps=[tu, ta, astfree[j_]])
            psfree[iu] = tv
            sgfree[i_] = tv
            astfree[j_] = P.op("sp", lambda e, fc=fc, j_=j_: e.dma_start(out=AT[fc, :, :], in_=AST[j_][:, :]),
                               deps=[tv], dsem=ast_sem[j_])
        P.barrier()
        for i in range(4):
            psfree[i] = None

    xld = [P.dma_sem(f"xld{i}") for i in range(3)]
    xst = [P.dma_sem(f"xst{i}") for i in range(3)]
    atl = [P.dma_sem(f"atl{i}") for i in range(4)]

    def stage_resid(xsrc, parts, scale, load_at):
        XT = [FT[0], FT[1], FT[2]]
        SQ, ACC = FT[3], FT[4]
        xfree = [None] * 3
        sqfree = [None] * 3
        acc_tok = [None]
        stok = {}
        fc0 = 0
        for pi, ps_ in enumerate(parts):
            atok = None
            ktok = {}
            if load_at:
                k0 = 0
                di = 0
                while k0 < ps_:
                    k1 = min(ps_, k0 + 8)
                    t_ = P.op("sp", lambda e, k0=k0, k1=k1, fc0=fc0: e.dma_start(
                        out=ACTB[:, k0:k1, :], in_=AT[fc0 + k0:fc0 + k1, :, :].rearrange("c p t -> p c t")),
                        dsem=atl[di % 4])
                    for kk in range(k0, k1):
                        ktok[kk] = t_
                    k0 = k1
                    di += 1
            src = xsrc if pi == 0 else XS
            ld = {}

            def load(dc):
                s_ = dc % 3
                ld[dc] = P.op("sp", lambda e, dc=dc, s_=s_, src=src: e.dma_start(out=XT[s_][:, :], in_=src[dc, :, :]),
                              deps=[xfree[s_], sqfree[s_], stok.get(dc)], dsem=xld[s_])
            for dc in range(min(2, KC)):
                load(dc)
            for dc in range(KC):
                if dc + 2 < KC:
                    load(dc + 2)
                slot, ltok = w_next()
                ip = dc % 4
                tm = mm_group(PS[ip], slot, 0, ps_, lambda kc: ACTB[:, kc, :], [ltok, psfree[ip], atok],
                              kdeps=((lambda kc: ktok.get(kc)) if (load_at and dc == 0) else None))
                w_issue(tm)
                s_ = dc % 3
                tv = P.op("dve", lambda e, ip=ip, s_=s_: e.scalar_tensor_tensor(
                    out=XT[s_][:, :], in0=PS[ip][:, :], scalar=float(scale), in1=XT[s_][:, :],
                    op0=ALU.mult, op1=ALU.add), deps=[tm, ld[dc]])
                psfree[ip] = tv
                stok[dc] = P.op("sp", lambda e, dc=dc, s_=s_: e.dma_start(out=XS[dc, :, :], in_=XT[s_][:, :]),
                                deps=[tv], dsem=xst[s_])
                xfree[s_] = stok[dc]
                if pi == len(parts) - 1:
                    if dc == 0:
                        t1 = P.op("act", lambda e, s_=s_: e.activation(out=ACC[:, :], in_=XT[s_][:, :], func=AF.Square),
                                  deps=[tv])
                        acc_tok[0] = t1
                    else:
                        t1 = P.op("act", lambda e, s_=s_: e.activation(out=SQ[:, :], in_=XT[s_][:, :], func=AF.Square),
                                  deps=[tv, acc_tok[0]])
                        acc_tok[0] = P.op("dve", lambda e: e.tensor_tensor(out=ACC[:, :], in0=ACC[:, :], in1=SQ[:, :],
                                                                           op=ALU.add), deps=[t1, acc_tok[0]])
                    sqfree[s_] = t1
            fc0 += ps_
            P.barrier()
            for i in range(4):
                psfree[i] = None
            for i in range(3):
                xfree[i] = None

    pst = [P.dma_sem(f"pst{i}") for i in range(2)]
    ml = [P.dma_sem(f"ml{i}") for i in range(8)]
    mst = [P.dma_sem(f"mst{i}") for i in range(4)]
    exs = P.dma_sem("exs")
    exl = P.dma_sem("exl")
    cvl = P.dma_sem("cvl")
    ccs = [es.enter_context(nc.semaphore(f"cc{l}")) for l in range(DEPTH)]
    L0, L1, T0, T1, T2, T3, T4 = FT
    KB, VB, KZ, VT, QB, QX, ST = BT
    gam = [1.0 - 2.0 ** (-5 - h) for h in range(H)]
    g128 = [float(np.float64(g) ** 128) for g in gam]

    def blk(t, n):
        return t[:, n * 128:(n + 1) * 128]

    RES = {}

    def _res(k):
        if k not in RES:
            RES[k] = [None, []]
        return RES[k]

    def _deps(reads, writes, extra=()):
        deps = list(extra)
        for k in reads:
            deps.append(_res(k)[0])
        for k in writes:
            r = _res(k)
            deps.append(r[0])
            deps += r[1]
        return deps

    def _upd(tok, reads, writes):
        if tok is None:
            return
        for k in reads:
            _res(k)[1].append(tok)
        for k in writes:
            r = _res(k)
            r[0] = tok
            r[1] = []

    def auto(eng, fn, reads=(), writes=(), dsem=None, extra=()):
        tok = P.op(eng, fn, _deps(reads, writes, extra), ms=(None if dsem is None else False), dsem=dsem)
        _upd(tok, reads, writes)
        return tok

    def auto_group(fns, reads=(), writes=(), extra=()):
        deps = _deps(reads, writes, extra)
        tok = None
        if c.pedrain:
            P.op("pe", lambda e: e.drain(), deps, ms=False)
            deps = ()
        for i, fn in enumerate(fns):
            tok = P.op("pe", fn, deps if i == 0 else (), ms=(i == len(fns) - 1))
        if c.pedrain:
            P.op("pe", lambda e: e.drain(), (), ms=False)
        _upd(tok, reads, writes)
        return tok

    def res_reset():
        RES.clear()

    order_in = ([H + h for h in range(H)] + [2 * H + h for h in range(H)] + [h for h in range(H)] +
                [5 * H + j for j in range(H)] + [6 * H + j for j in range(H)] + [4 * H + j for j in range(H)] +
                [3 * H + h for h in range(H)])

    def rope_bg(src, dst, psname, ps):
        auto_group([lambda e, hf=hf: e.matmul(out=ps[:, hf * 512:(hf + 1) * 512], lhsT=con(c.c_perm, 128),
                                              rhs=src[1][:, hf * 512:(hf + 1) * 512], start=True, stop=True)
                    for hf in range(NH)], reads=[src[0]], writes=[psname])
        yield
        auto("dve", lambda e: e.tensor_tensor(out=T0[:, :], in0=src[1][:, :], in1=con(c.c_cos, TT), op=ALU.mult),
             reads=[src[0]], writes=["T0"])
        auto("dve", lambda e: e.tensor_tensor(out=T1[:, :], in0=ps[:, :], in1=con(c.c_sin, TT), op=ALU.mult),
             reads=[psname], writes=["T1"])
        auto("dve", lambda e: e.tensor_tensor(out=dst[1][:, :], in0=T0[:, :], in1=T1[:, :], op=ALU.add),
             reads=["T0", "T1"], writes=[dst[0]])

    def bg_job(l, pt_ready):
        pk = PS[1][:, 0:TT // 2].bitcast(BF16)
        pv = PS[1][:, TT // 2:TT].bitcast(BF16)
        alldone = lambda: all(pt_ready(cc) for cc in range(NP))
        kv_loaded = set()

        def load_kv(h):
            kv_loaded.add(h)
            auto("sp", lambda e, h=h: e.dma_start(out=L0[:, :], in_=PT[H + h, :, :]), reads=[("pt", H + h)],
                 writes=["L0"], dsem=ml[0])
            auto("sp", lambda e, h=h: e.dma_start(out=L1[:, :], in_=PT[2 * H + h, :, :]), reads=[("pt", 2 * H + h)],
                 writes=["L1"], dsem=ml[1])

        for h in range(H):
            while c.gate in (1, 4) and h >= (1 if c.gate == 1 else 0) and not alldone():
                yield
            while not (pt_ready(H + h) and pt_ready(2 * H + h)):
                yield
            if h not in kv_loaded:
                load_kv(h)
            yield from rope_bg(("L0", L0), ("T2", T2), "PS0", PS[0])
            q_early = pt_ready(h)
            if q_early:
                auto("sp", lambda e, h=h: e.dma_start(out=L0[:, :], in_=PT[h, :, :]), reads=[("pt", h)], writes=["L0"],
                     dsem=ml[0])
            auto("act", lambda e: e.activation(out=KB[:, :], in_=T2[:, :], func=AF.Copy), reads=["T2"], writes=["KB"])
            auto("act", lambda e: e.activation(out=VB[:, :], in_=L1[:, :], func=AF.Copy), reads=["L1"], writes=["VB"])
            if not (c.bgskip & 8 and h >= 1):
                auto_group([lambda e, n=n: e.transpose(out=blk(pk, n), in_=blk(KB, n), identity=IDB[:, :]) for n in range(NCH)] +
                           [lambda e, n=n: e.transpose(out=blk(pv, n), in_=blk(VB, n), identity=IDB[:, :]) for n in range(NCH)],
                           reads=["KB", "VB"], writes=["PS1"])
            yield
            if not (c.bgskip & 1 and h >= 1):
                auto("dve", lambda e, h=h: e.tensor_scalar(out=KZ[:, :], in0=pk, scalar1=con(c.c_zeta + h), scalar2=None,
                                                           op0=ALU.mult), reads=["PS1"], writes=["KZ"])
            if not (c.bgskip & 2 and h >= 1):
                auto("act", lambda e: e.activation(out=VT[:, :], in_=pv, func=AF.Copy), reads=["PS1"], writes=["VT"])
            if c.kvbar and h >= 1:
                P.barrier()
            if not (c.bgskip & 4 and h >= 1):
                auto_group([lambda e, n=n: e.matmul(out=blk(PS[1], n), lhsT=blk(KZ, n), rhs=blk(VT, n), start=True, stop=True)
                            for n in range(NCH)], reads=["KZ", "VT"], writes=["PS1"])
            if not q_early:
                yield
            auto("act", lambda e: e.activation(out=T2[:, :], in_=PS[1][:, :], func=AF.Copy), reads=["PS1"], writes=["T2"])
            auto("sp", lambda e, h=h: e.dma_start(out=KVS[h, :, :], in_=T2[:, :]), reads=["T2"], writes=[("kvs", h)],
                 dsem=mst[0])
            auto("dve", lambda e, h=h: e.tensor_copy(out=blk(SIN, h), in_=blk(T2, 0)), reads=["T2"], writes=["SIN"])
            for n in range(1, NCH):
                auto("dve", lambda e, h=h, n=n: e.scalar_tensor_tensor(
                    out=blk(SIN, h), in0=blk(SIN, h), scalar=g128[h], in1=blk(T2, n),
                    op0=ALU.mult, op1=ALU.add), reads=["T2", "SIN"], writes=["SIN"])
            while c.gate == 2 and h >= 1 and not alldone():
                yield
            if not q_early:
                while not pt_ready(h):
                    yield
                auto("sp", lambda e, h=h: e.dma_start(out=L0[:, :], in_=PT[h, :, :]), reads=[("pt", h)], writes=["L0"],
                     dsem=ml[0])
            yield from rope_bg(("L0", L0), ("L1", L1), "PS0", PS[0])
            auto("act", lambda e: e.activation(out=QB[:, :], in_=L1[:, :], func=AF.Copy), reads=["L1"], writes=["QB"])
            auto("dve", lambda e, h=h: e.tensor_tensor(
                out=QX[:, :].rearrange("p (n c) -> p n c", c=128), in0=L1[:, :].rearrange("p (n c) -> p n c", c=128),
                in1=con(c.c_xi + h * 128, 128).unsqueeze(1).to_broadcast([128, NCH, 128]), op=ALU.mult),
                 reads=["L1"], writes=["QX"])
            auto("sp", lambda e, h=h: e.dma_start(out=QXS[h, :, :], in_=QX[:, :]), reads=["QX"], writes=[("qxs", h)],
                 dsem=mst[1])
            auto_group([lambda e, n=n: e.matmul(out=blk(PS[1], n), lhsT=blk(KB, n), rhs=blk(QB, n), start=True, stop=True)
                        for n in range(NCH)], reads=["KB", "QB"], writes=["PS1"])
            if h + 1 < H and pt_ready(H + h + 1) and pt_ready(2 * H + h + 1):
                load_kv(h + 1)
            yield
            auto("dve", lambda e, h=h: e.tensor_tensor(
                out=ST[:, :].rearrange("p (n c) -> p n c", c=128), in0=PS[1][:, :].rearrange("p (n c) -> p n c", c=128),
                in1=con(c.c_dm + h * 128, 128).unsqueeze(1).to_broadcast([128, NCH, 128]), op=ALU.mult),
                 reads=["PS1"], writes=["ST"])
            auto_group([lambda e, n=n: e.matmul(out=blk(PS[0], n), lhsT=blk(VT, n), rhs=blk(ST, n), start=True, stop=True)
                        for n in range(NCH)], reads=["VT", "ST"], writes=["PS0"])
            yield
            auto("act", lambda e: e.activation(out=T3[:, :], in_=PS[0][:, :], func=AF.Copy), reads=["PS0"], writes=["T3"])
            auto("sp", lambda e, h=h: e.dma_start(out=INTRA[h, :, :], in_=T3[:, :]), reads=["T3"], writes=[("intra", h)],
                 dsem=mst[2])
        while c.convlate and not all(pt_ready(cc) for cc in range(NP)):
            yield
        auto("dve", lambda e: e.memset(CU[:, 0:2], 0.0), writes=["CU"])
        for j in range(H):
            while not (pt_ready(4 * H + j) and pt_ready(5 * H + j) and pt_ready(6 * H + j)):
                yield
            auto("sp", lambda e, j=j: e.dma_start(out=L0[:, :], in_=PT[5 * H + j, :, :]), reads=[("pt", 5 * H + j)],
                 writes=["L0"], dsem=ml[0])
            auto("sp", lambda e, j=j: e.dma_start(out=L1[:, :], in_=PT[6 * H + j, :, :]), reads=[("pt", 6 * H + j)],
                 writes=["L1"], dsem=ml[1])
            auto("sp", lambda e, j=j: e.dma_start(out=T2[:, :], in_=PT[4 * H + j, :, :]), reads=[("pt", 4 * H + j)],
                 writes=["T2"], dsem=ml[2])
            auto("dve", lambda e: e.tensor_tensor(out=CU[:, 2:TT + 2], in0=L0[:, :], in1=L1[:, :], op=ALU.mult),
                 reads=["L0", "L1"], writes=["CU"])
            cw = lambda tap, j=j: con(c.c_cw + (l * 3 + tap) * H + j)
            auto("dve", lambda e, j=j: e.tensor_copy(out=HALO[:, j, :], in_=CU[:, TT:TT + 2]), reads=["CU"], writes=["HALO"])
            auto("dve", lambda e, cw=cw: e.tensor_scalar(out=T0[:, :], in0=CU[:, 2:TT + 2], scalar1=cw(2), scalar2=None,
                                                        op0=ALU.mult), reads=["CU"], writes=["T0"])
            auto("dve", lambda e, cw=cw: e.scalar_tensor_tensor(out=T0[:, :], in0=CU[:, 1:TT + 1], scalar=cw(1),
                                                               in1=T0[:, :], op0=ALU.mult, op1=ALU.add),
                 reads=["CU", "T0"], writes=["T0"])
            auto("dve", lambda e, cw=cw: e.scalar_tensor_tensor(out=T0[:, :], in0=CU[:, 0:TT], scalar=cw(0),
                                                               in1=T0[:, :], op0=ALU.mult, op1=ALU.add),
                 reads=["CU", "T0"], writes=["T0"])
            auto("dve", lambda e, j=j: e.tensor_copy(out=Y2[:, j, :], in_=T0[:, 0:2]), reads=["T0"], writes=["Y2"])
            auto("dve", lambda e, j=j: e.tensor_copy(out=B2[:, j, :], in_=T2[:, 0:2]), reads=["T2"], writes=["B2"])
            auto("dve", lambda e: e.tensor_tensor(out=KB[:, :], in0=T0[:, :], in1=T2[:, :], op=ALU.mult),
                 reads=["T0", "T2"], writes=["KB"])
            auto("sp", lambda e, j=j: e.dma_start(out=CVO[j, :, :], in_=KB[:, :]), reads=["KB"], writes=[("cvo", j)],
                 dsem=mst[3])
            yield

    def stage_mix(l):
        res_reset()
        ptok = {}
        ready = lambda cc: cc in ptok
        bg = bg_job(l, ready)
        last_tm = [None]
        st_ = {"alive": not c.nobg}

        def adv():
            st_["n"] = st_.get("n", 0) + 1
            if st_["n"] > c.bgsteps:
                st_["alive"] = False
            if st_["alive"]:
                try:
                    next(bg)
                except StopIteration:
                    st_["alive"] = False

        for i, cc in enumerate(order_in):
            slot, ltok = w_next()
            ip = 2 + (i % 2)
            psn = f"PS{ip}"
            deps = _deps([], [psn], [ltok])
            tm = mm_group(PS[ip], slot, 0, KC, lambda kc: ACTB[:, kc, :], deps, mid=(adv if (i >= 2 * H and c.bg2) else None))
            _upd(tm, [], [psn])
            last_tm[0] = tm
            w_issue(tm)
            if i % 2 == 0:
                auto("act", lambda e, ip=ip: e.activation(out=T4[:, :], in_=PS[ip][:, :], func=AF.Copy),
                     reads=[psn], writes=["T4"])
            else:
                auto("dve", lambda e, ip=ip: e.tensor_copy(out=T4[:, :], in_=PS[ip][:, :]), reads=[psn], writes=["T4"])
            ptok[cc] = auto("sp", lambda e, cc=cc: e.dma_start(out=PT[cc, :, :], in_=T4[:, :]), reads=["T4"],
                            writes=[("pt", cc)], dsem=pst[i % 2])
            if i >= 2 * H - 1:
                for _ in range(c.bgn):
                    adv()
        while st_["alive"]:
            adv()
        if c.stopat == 1:
            raise StopBuild()
        auto("sp", lambda e: e.dma_start(out=ACTB[:, H:2 * H, :], in_=CVO[:, :, :].rearrange("c p t -> p c t")),
             reads=[("cvo", j) for j in range(H)], writes=["ACTBc"], dsem=cvl, extra=[last_tm[0]])
        sets = [dict(I=("L0", L0), K=("L1", L1), G=("T0", T0), Q=("KB", KB), ps=("PS0", PS[0]), pn=("PS2", PS[2])),
                dict(I=("T1", T1), K=("T2", T2), G=("T3", T3), Q=("VB", VB), ps=("PS1", PS[1]), pn=("PS3", PS[3]))]

        def loads_kq(h):
            s_ = sets[h % 2]
            o = 4 * (h % 2)
            auto("sp", lambda e: e.dma_start(out=s_["K"][1][:, :], in_=KVS[h, :, :]), reads=[("kvs", h)], writes=[s_["K"][0]], dsem=ml[o + 1])
            auto("sp", lambda e: e.dma_start(out=s_["Q"][1][:, :], in_=QXS[h, :, :]), reads=[("qxs", h)], writes=[s_["Q"][0]], dsem=ml[o + 2])

        def loads_ig(h):
            s_ = sets[h % 2]
            o = 4 * (h % 2)
            auto("sp", lambda e: e.dma_start(out=s_["I"][1][:, :], in_=INTRA[h, :, :]), reads=[("intra", h)], writes=[s_["I"][0]], dsem=ml[o + 0])
            auto("sp", lambda e: e.dma_start(out=s_["G"][1][:, :], in_=PT[3 * H + h, :, :]), reads=[("pt", 3 * H + h)], writes=[s_["G"][0]], dsem=ml[o + 3])

        loads_kq(0)
        loads_ig(0)
        if H > 1:
            loads_kq(1)
        auto("dve", lambda e: e.tensor_copy(out=SIN[:, H * 128:c.EW].rearrange("p (c t) -> p c t", t=2), in_=HALO[:, :, :]),
             reads=["HALO", "SIN"], writes=["SIN"])
        ts = auto("sp", lambda e: e.dma_start(out=EXI[l][:, :], in_=SIN[:, :]), reads=["SIN"], writes=["EXI"], dsem=exs)
        P.op("pool", lambda e: e.collective_compute(
            "AllGather", ALU.bypass, replica_groups=[[2 * i, 2 * i + 1] for i in range(NCORES // 2)],
            ins=[EXI[l].ap().opt()], outs=[EXO[l].ap().opt()]).then_inc(ccs[l]), deps=[ts], ms=False)
        P.op("pool", lambda e: e.wait_ge(ccs[l], 1), ms=False)
        tl = P.op("pool", lambda e: e.dma_start(out=SIN[:, :], in_=EXO[l][0:128, :]), dsem=exl)
        P.barrier()
        res_reset()
        auto("dve", lambda e: e.tensor_scalar(out=SIN[:, :], in0=SIN[:, :], scalar1=con(c.c_sel), scalar2=None,
                                              op0=ALU.mult), writes=["SIN"], extra=[tl])
        if c.stopat == 2:
            raise StopBuild()
        HL = SIN[:, H * 128:c.EW].rearrange("p (c t) -> p c t", t=2)
        W0 = con(c.c_cw + (l * 3 + 0) * H, H)
        W1 = con(c.c_cw + (l * 3 + 1) * H, H)
        auto("dve", lambda e: e.tensor_tensor(out=FX[:, :, 0], in0=HL[:, :, 1], in1=W1, op=ALU.mult), reads=["SIN"], writes=["FX"])
        auto("dve", lambda e: e.tensor_tensor(out=FX[:, :, 1], in0=HL[:, :, 0], in1=W0, op=ALU.mult), reads=["SIN"], writes=["FX"])
        auto("dve", lambda e: e.tensor_tensor(out=FX[:, :, 0], in0=FX[:, :, 0], in1=FX[:, :, 1], op=ALU.add), reads=["FX"], writes=["FX"])
        auto("dve", lambda e: e.tensor_tensor(out=FX[:, :, 1], in0=HL[:, :, 1], in1=W0, op=ALU.mult), reads=["SIN"], writes=["FX"])
        auto("dve", lambda e: e.tensor_tensor(out=Y2[:, :, :], in0=Y2[:, :, :], in1=FX[:, :, :], op=ALU.add), reads=["FX"], writes=["Y2"])
        auto("dve", lambda e: e.tensor_tensor(out=ACTB[:, H:2 * H, 0:2], in0=Y2[:, :, :], in1=B2[:, :, :], op=ALU.mult),
             reads=["Y2", "ACTBc"], writes=["ACTBc"])
        if c.stopat == 3:
            raise StopBuild()
        def part1(h):
            s_ = sets[h % 2]
            Kn, Kt = s_["K"]
            auto("dve", lambda e: e.tensor_copy(out=blk(SST, 0), in_=blk(SIN, h)), reads=["SIN"], writes=["SST"])
            for n in range(NCH):
                auto("dve", lambda e, n=n: e.scalar_tensor_tensor(
                    out=blk(SST, n + 1), in0=blk(SST, n), scalar=g128[h], in1=blk(Kt, n),
                    op0=ALU.mult, op1=ALU.add), reads=["SST", Kn], writes=["SST"])
            auto("act", lambda e: e.activation(out=SBALL[:, :], in_=SST[:, 0:NCH * 128], func=AF.Copy),
                 reads=["SST"], writes=["SBALL"])
            psn, ps = s_["ps"]
            Qn, Qt = s_["Q"]
            auto_group([lambda e, n=n: e.matmul(out=blk(ps, n), lhsT=blk(SBALL, n), rhs=blk(Qt, n), start=True, stop=True)
                        for n in range(NCH)], reads=["SBALL", Qn], writes=[psn])

        def part2(h):
            s_ = sets[h % 2]
            In, It = s_["I"]
            Gn, Gt = s_["G"]
            psn, ps = s_["ps"]
            pnn, pn = s_["pn"]
            auto("dve", lambda e: e.tensor_tensor(out=It[:, :], in0=ps[:, :], in1=It[:, :], op=ALU.add),
                 reads=[psn, In], writes=[In])
            auto("act", lambda e: e.activation(out=T4[:, :], in_=It[:, :], func=AF.Square), reads=[In], writes=["T4"])
            auto_group([lambda e, hf=hf: e.matmul(out=pn[:, hf * 512:(hf + 1) * 512], lhsT=con(c.c_onesh, 128),
                                                  rhs=T4[:, hf * 512:(hf + 1) * 512], start=True, stop=True)
                        for hf in range(NH)], reads=["T4"], writes=[pnn])
            auto("act", lambda e: e.activation(out=T4[:, :], in_=pn[:, :], func=AF.Sqrt, bias=con(c.c_eps), scale=1.0),
                 reads=[pnn], writes=["T4"])
            auto("act", lambda e: e.activation(out=Gt[:, :], in_=Gt[:, :], func=AF.Silu), reads=[Gn], writes=[Gn])
            auto("dve", lambda e: e.reciprocal(out=T4[:, :], in_=T4[:, :]), reads=["T4"], writes=["T4"])
            auto("dve", lambda e: e.tensor_tensor(out=It[:, :], in0=It[:, :], in1=T4[:, :], op=ALU.mult),
                 reads=[In, "T4"], writes=[In])
            auto("dve", lambda e: e.scalar_tensor_tensor(
                out=ACTB[:, h, :], in0=It[:, :], scalar=con(c.c_rn + l * H + h), in1=Gt[:, :],
                op0=ALU.mult, op1=ALU.mult), reads=[In, Gn], writes=[("actb", h)])

        for h in range(H + 1):
            if 1 <= h and h + 1 < H:
                loads_kq(h + 1)
            if h < H:
                part1(h)
            if h >= 1:
                part2(h - 1)
            if h + 1 < H:
                loads_ig(h + 1)
            if c.pbser:
                P.barrier()
        P.barrier()
        res_reset()

    dbs = P.dma_sem("dbs")

    def dump_r():
        P.op("sp", lambda e: e.dma_start(out=DBR[:, :, :].rearrange("c p t -> p c t"), in_=ACTB[:, :, :]), dsem=dbs)
        P.barrier()

    xcur = xin
    try:
      for l in range(DEPTH):
          stage_norm(xcur, 3 * l + 0, have_acc=(l > 0))
          stage_gateup()
          stage_resid(xcur, c.parts, 0.5, True)
          xcur = XS
          if c.mixer:
              stage_norm(xcur, 3 * l + 1, have_acc=True)
              stage_mix(l)
              if c.debug and l == 0:
                  dump_r()
              stage_resid(xcur, [KC], 1.0, False)
          stage_norm(xcur, 3 * l + 2, have_acc=c.mixer)
          stage_gateup()
          stage_resid(xcur, c.parts, 0.5, True)
      stage_norm(xcur, 3 * DEPTH, final=True, have_acc=True)
      assert wstate["consumed"] == len(wchunks), (wstate, len(wchunks))
    except StopBuild:
      P.barrier()

    assert P.simulate(), "deadlock in program order"
    with nc.Block() as block:
        P.emit(block)
    es.close()
    return nc


def _consts(cfg, half, norms, ret_norm, conv_w):
    c = cfg
    H, TT = c.H, c.TT
    A = np.zeros((128, c.CW), np.float32)
    A[:, c.c_ident:c.c_ident + 128] = np.eye(128, dtype=np.float32)
    perm = np.zeros((128, 128), np.float32)
    for dp in range(128):
        perm[(dp + 64) % 128, dp] = 1.0
    A[:, c.c_perm:c.c_perm + 128] = perm
    A[:, c.c_onesd:c.c_onesd + 128] = 1.0 / c.D
    A[:, c.c_onesh:c.c_onesh + 128] = 1.0 / 128.0
    half_d = 64
    inv_freq = (10000.0 ** (-np.arange(half_d, dtype=np.float32) / np.float32(half_d))).astype(np.float32)
    pos = (half * TT + np.arange(TT)).astype(np.float32)
    ang = (pos[None, :] * inv_freq[:, None]).astype(np.float32)
    cos = np.cos(ang).astype(np.float32)
    sin = np.sin(ang).astype(np.float32)
    A[:, c.c_cos:c.c_cos + TT] = np.concatenate([cos, cos], 0)
    A[:, c.c_sin:c.c_sin + TT] = np.concatenate([-sin, sin], 0)
    hh = np.arange(H, dtype=np.float64)
    lg = np.log1p(-np.power(2.0, -5.0 - hh))
    idx = np.arange(128, dtype=np.float64)
    scale = 128.0 ** -0.5
    diff = idx[None, :] - idx[:, None]
    dm = np.where(diff[None] >= 0, np.exp(lg[:, None, None] * np.maximum(diff, 0.0)[None]), 0.0) * scale
    A[:, c.c_dm:c.c_dm + H * 128] = dm.transpose(1, 0, 2).reshape(128, H * 128)
    xi = np.exp(lg[:, None] * (idx + 1.0)[None, :])
    A[:, c.c_xi:c.c_xi + H * 128] = np.broadcast_to(xi.reshape(1, H * 128), (128, H * 128))
    zeta = np.exp(lg[:, None] * (127.0 - idx)[None, :]) * scale
    A[:, c.c_zeta:c.c_zeta + H] = zeta.T
    A[:, c.c_sel] = float(half)
    A[:, c.c_eps] = EPS
    for i, g in enumerate(norms):
        A[:, c.c_norm + i * c.KC:c.c_norm + (i + 1) * c.KC] = g.reshape(c.KC, 128).T
    for l in range(c.DEPTH):
        A[:, c.c_rn + l * H:c.c_rn + (l + 1) * H] = ret_norm[l].reshape(H, 128).T
        for tap in range(3):
            o = c.c_cw + (l * 3 + tap) * H
            A[:, o:o + H] = conv_w[l, tap].reshape(H, 128).T
    return A


def _lay_cols(w, KC):
    K, N = w.shape
    return np.ascontiguousarray(w.reshape(KC, 128, N // 128, 128).transpose(2, 1, 0, 3)).reshape(N // 128, 128, KC * 128)


def prepare_inputs(cfg, x, norm_ffa, w_ffa_gate, w_ffa_up, w_ffa_down, norm_mix, w_in, conv_w, ret_norm, w_out,
                   norm_ffb, w_ffb_gate, w_ffb_up, w_ffb_down, norm_final):
    c = cfg
    KC, FC, TT = c.KC, c.FC, c.TT
    shared = {}
    for l in range(c.DEPTH):
        for f, (wg, wu, wdn) in (("a", (w_ffa_gate, w_ffa_up, w_ffa_down)), ("b", (w_ffb_gate, w_ffb_up, w_ffb_down))):
            g = _lay_cols(np.asarray(wg[l]), KC).reshape(FC, 128, 1, KC * 128)
            u = _lay_cols(np.asarray(wu[l]), KC).reshape(FC, 128, 1, KC * 128)
            shared[f"wgu_{l}{f}"] = np.concatenate([g, u], axis=2).reshape(FC, 128, 2 * KC * 128)
            wdl = np.zeros((3 * KC, 128, c.PSMAX * 128), np.float32)
            r0 = 0
            wdn_l = np.asarray(wdn[l])
            for pi, ps_ in enumerate(c.parts):
                blk = wdn_l[r0 * 128:(r0 + ps_) * 128]
                wdl[pi * KC:(pi + 1) * KC, :, 0:ps_ * 128] = _lay_cols(blk, ps_)
                r0 += ps_
            shared[f"wd_{l}{f}"] = wdl
        shared[f"win_{l}"] = _lay_cols(np.asarray(w_in[l]), KC)
        shared[f"wout_{l}"] = _lay_cols(np.asarray(w_out[l]), KC)
    norms = []
    for l in range(c.DEPTH):
        norms += [np.asarray(norm_ffa[l]), np.asarray(norm_mix[l]), np.asarray(norm_ffb[l])]
    norms.append(np.asarray(norm_final))
    x = np.asarray(x)
    in_maps = []
    for core in range(NCORES):
        b, half = core // 2, core % 2
        xt = np.ascontiguousarray(x[b, half * TT:(half + 1) * TT, :].T).reshape(KC, 128, TT)
        m = dict(shared)
        m["xin"] = xt
        m["consts"] = _consts(c, half, norms, np.asarray(ret_norm), np.asarray(conv_w))
        in_maps.append(m)
    return in_maps


def run(cfg, inputs, trace=False):
    nc = build_nc(cfg)
    in_maps = prepare_inputs(cfg, **inputs)
    res = run_bass_kernel_spmd(nc, in_maps, core_ids=list(range(NCORES)), **({"trace": True} if trace else {}))
    B = NCORES // 2
    out = np.zeros((B, 2 * cfg.TT, cfg.D), np.float32)
    for core in range(NCORES):
        b, half = core // 2, core % 2
        yv = np.asarray(res.results[core]["y"]).reshape(cfg.D, cfg.TT)
        out[b, half * cfg.TT:(half + 1) * cfg.TT, :] = yv.T
    return out, res


def kernel(**inputs):
    cfg = Cfg()
    out, _ = run(cfg, inputs)
    return out
```
